# Optimizing a Trainium2 kernel written in Bass

```python
import jax, jax.numpy as jnp
from jax import lax
import numpy as np

D_MODEL = 1024
BATCH = 4
SEQ = 8192
DEPTH = 1

CHUNK = 64
EPS = 1e-6

LRU_WIDTH = D_MODEL
LRU_HEADS = 8
LRU_HEAD_DIM = LRU_WIDTH // LRU_HEADS
LRU_CONV = 4
LRU_C = 8.0

CONV_WIDTH = D_MODEL
CONV_GROUPS = 8
CONV_K = 3

N_BRANCHES = 2
IN_COLS = 2 * LRU_WIDTH + 3 * CONV_WIDTH + N_BRANCHES * D_MODEL

PEER_HEADS = 8
PEER_NKEYS = 128
PEER_N = PEER_NKEYS * PEER_NKEYS
PEER_DKEY = 256
PEER_HALF = PEER_DKEY // 2
PEER_TOPK = 16
PEER_BLOCK = 128

kernel_name = "hybrid_rglru_shortconv_peer"


def rmsnorm(x, g):
    xf = x.astype(jnp.float32)
    y = xf * lax.rsqrt(jnp.mean(xf * xf, axis=-1, keepdims=True) + EPS)
    return (y * g.astype(jnp.float32)).astype(x.dtype)


def causal_dwconv(x, w):
    k, c = w.shape
    return lax.conv_general_dilated(
        x, w[:, None, :].astype(x.dtype), window_strides=(1,), padding=[(k - 1, 0)],
        dimension_numbers=("NWC", "WIO", "NWC"), feature_group_count=c)


def rg_lru(x, w_a, b_a, w_x, b_x, lam):
    b, s, _ = x.shape
    xh = x.reshape(b, s, LRU_HEADS, LRU_HEAD_DIM)
    r = jax.nn.sigmoid(jnp.einsum("bshi,hij->bshj", xh, w_a).reshape(b, s, LRU_WIDTH) + b_a)
    i = jax.nn.sigmoid(jnp.einsum("bshi,hij->bshj", xh, w_x).reshape(b, s, LRU_WIDTH) + b_x)
    log_a = (-LRU_C * r.astype(jnp.float32)) * jax.nn.softplus(-lam.astype(jnp.float32))
    a = jnp.exp(log_a)
    mult = jnp.sqrt(-jnp.expm1(2.0 * log_a))
    u = mult * (i * x).astype(jnp.float32)

    def combine(left, right):
        a1, b1 = left
        a2, b2 = right
        return a1 * a2, a2 * b1 + b2

    _, h = lax.associative_scan(combine, (a, u), axis=1)
    return h.astype(x.dtype)


def token_mixers(xn, w_in, conv_a_w, conv_a_b, w_a, b_a, w_x, b_x, lam, conv_b_w, w_out):
    p = xn @ w_in
    splits = [LRU_WIDTH, 2 * LRU_WIDTH, 2 * LRU_WIDTH + CONV_WIDTH,
              2 * LRU_WIDTH + 2 * CONV_WIDTH, 2 * LRU_WIDTH + 3 * CONV_WIDTH,
              2 * LRU_WIDTH + 3 * CONV_WIDTH + D_MODEL]
    xa, ga, vb, bb, cb, ma, mb = jnp.split(p, splits, axis=-1)
    ya = rg_lru(causal_dwconv(xa, conv_a_w) + conv_a_b, w_a, b_a, w_x, b_x, lam) * jax.nn.gelu(ga)
    yb = bb * causal_dwconv(cb * vb, conv_b_w)
    merged = jax.nn.sigmoid(ma) * ya + jax.nn.sigmoid(mb) * yb
    return merged @ w_out


def peer(xn, w_q, sub_keys, u_tab, v_tab):
    b, s, d = xn.shape
    t = xn.reshape(-1, d)
    n_tok = t.shape[0]
    q = (t @ w_q).reshape(n_tok, PEER_HEADS, 2, PEER_HALF)
    sc = jnp.einsum("thpk,hpnk->thpn", q, sub_keys).astype(jnp.float32)
    s_top, i_top = lax.top_k(sc, PEER_TOPK)
    cand = s_top[:, :, 0, :, None] + s_top[:, :, 1, None, :]
    cand_idx = i_top[:, :, 0, :, None] * PEER_NKEYS + i_top[:, :, 1, None, :]
    kk = PEER_TOPK * PEER_TOPK
    best, pos = lax.top_k(cand.reshape(n_tok, PEER_HEADS, kk), PEER_TOPK)
    idx = jnp.take_along_axis(cand_idx.reshape(n_tok, PEER_HEADS, kk), pos, axis=-1)
    g = jax.nn.softmax(best, axis=-1).astype(xn.dtype)

    nb = n_tok // PEER_BLOCK

    def block(args):
        tb, ib, gb = args
        ue = u_tab[ib]
        act = jax.nn.gelu(jnp.einsum("phkd,pd->phk", ue, tb))
        ve = v_tab[ib]
        return jnp.einsum("phk,phkd->pd", gb * act, ve)

    out = lax.map(block, (t.reshape(nb, PEER_BLOCK, d),
                          idx.reshape(nb, PEER_BLOCK, PEER_HEADS, PEER_TOPK),
                          g.reshape(nb, PEER_BLOCK, PEER_HEADS, PEER_TOPK)))
    return out.reshape(b, s, d)


def setup_inputs(seed: int = 0) -> dict:
    key = jax.random.key(seed)
    ks = jax.random.split(key, 20)
    f = jnp.float32
    L = DEPTH

    def nrm(k, shape, scale):
        return jax.random.normal(k, shape, f) * scale

    x = nrm(ks[0], (BATCH, SEQ, D_MODEL), 1.0)
    norm1_g = 1.0 + nrm(ks[1], (L, D_MODEL), 0.02)
    w_in = nrm(ks[2], (L, D_MODEL, IN_COLS), D_MODEL ** -0.5)
    conv_a_w = nrm(ks[3], (L, LRU_CONV, LRU_WIDTH), LRU_CONV ** -0.5)
    conv_a_b = nrm(ks[4], (L, LRU_WIDTH), 0.01)
    w_a = nrm(ks[5], (L, LRU_HEADS, LRU_HEAD_DIM, LRU_HEAD_DIM), LRU_HEAD_DIM ** -0.5)
    b_a = nrm(ks[6], (L, LRU_WIDTH), 0.01)
    w_x = nrm(ks[7], (L, LRU_HEADS, LRU_HEAD_DIM, LRU_HEAD_DIM), LRU_HEAD_DIM ** -0.5)
    b_x = nrm(ks[8], (L, LRU_WIDTH), 0.01)
    a_init = jax.random.uniform(ks[9], (L, LRU_WIDTH), f, 0.9, 0.999)
    sig = a_init ** (1.0 / LRU_C)
    lru_lambda = jnp.log(sig) - jnp.log1p(-sig)
    conv_b_w = nrm(ks[10], (L, CONV_K, CONV_WIDTH), CONV_K ** -0.5)
    w_out = nrm(ks[11], (L, D_MODEL, D_MODEL), D_MODEL ** -0.5)
    norm2_g = 1.0 + nrm(ks[12], (L, D_MODEL), 0.02)
    peer_wq = nrm(ks[13], (L, D_MODEL, PEER_HEADS * PEER_DKEY), D_MODEL ** -0.5)
    peer_subkeys = nrm(ks[14], (L, PEER_HEADS, 2, PEER_NKEYS, PEER_HALF), PEER_HALF ** -0.5)
    peer_u = nrm(ks[15], (L, PEER_N, D_MODEL), D_MODEL ** -0.5)
    peer_v = nrm(ks[16], (L, PEER_N, D_MODEL), PEER_HEADS ** -0.5)
    final_g = 1.0 + nrm(ks[17], (D_MODEL,), 0.02)
    return {"x": x, "norm1_g": norm1_g, "w_in": w_in, "conv_a_w": conv_a_w, "conv_a_b": conv_a_b,
            "w_a": w_a, "b_a": b_a, "w_x": w_x, "b_x": b_x, "lru_lambda": lru_lambda,
            "conv_b_w": conv_b_w, "w_out": w_out, "norm2_g": norm2_g, "peer_wq": peer_wq,
            "peer_subkeys": peer_subkeys, "peer_u": peer_u, "peer_v": peer_v, "final_g": final_g}


def reference(x, norm1_g, w_in, conv_a_w, conv_a_b, w_a, b_a, w_x, b_x, lru_lambda,
              conv_b_w, w_out, norm2_g, peer_wq, peer_subkeys, peer_u, peer_v, final_g):
    h = x
    for l in range(DEPTH):
        xn = rmsnorm(h, norm1_g[l])
        h = h + token_mixers(xn, w_in[l], conv_a_w[l], conv_a_b[l], w_a[l], b_a[l], w_x[l], b_x[l],
                             lru_lambda[l], conv_b_w[l], w_out[l])
        h = h + peer(rmsnorm(h, norm2_g[l]), peer_wq[l], peer_subkeys[l], peer_u[l], peer_v[l])
    return rmsnorm(h, final_g)
```

```python
import numpy as np
from contextlib import ExitStack
import concourse.bass as bass
import concourse.mybir as mybir
from concourse.bass_utils import run_bass_kernel_spmd

F32 = mybir.dt.float32
BF16 = mybir.dt.bfloat16
U32 = mybir.dt.uint32
AF = mybir.ActivationFunctionType
ALU = mybir.AluOpType
AX = mybir.AxisListType

D = 1024
NT = 256
TOK = 4096
NTL = TOK // NT
NPRE = TOK // NT
JG = 4
NJG = 128 // JG
TB = 8
EPS = 1e-6
GELU_TANH = True
GELU_MANUAL = False
NO_INTERLEAVE = False
PRE_PULL = 0
STRICT_SYNC = False

K_G1, K_CAW, K_CAB, K_BA, K_BX, K_LAM, K_CBW, K_G2, K_GF, NPARAM = 0, 1, 5, 6, 7, 8, 9, 12, 13, 14


class _Rec:
    def __init__(self):
        self.call = None

    def __getattr__(self, name):
        def f(*a, **kw):
            self.call = (name, a, kw)
            return self
        return f


class _Op:
    __slots__ = ("e", "call", "deps", "need", "dma", "idx", "sigval")


class Sch:
    COMPUTE = ('pe', 'act', 'dve', 'pool')

    def __init__(self, nc, es):
        self.nc = nc
        self.es = es
        self.eng = {'pe': nc.tensor, 'act': nc.scalar, 'dve': nc.vector, 'pool': nc.gpsimd, 'sp': nc.sync}
        self.sems = {}
        self.sigcnt = {}
        self.issue = {}
        for e in self.eng:
            self.sems[e] = es.enter_context(nc.semaphore("s_" + e))
            self.sigcnt[e] = 0
            self.issue[e] = 0
        self.dcnt = {}
        self.waited = {e: {} for e in self.eng}
        self.lastw = {}
        self.readers = {}
        self.ops = []
        self.lastop = {}

    def op(self, e, fn, r=(), w=(), dma=None, sig=True):
        rec = _Rec()
        fn(rec)
        o = _Op()
        o.e = e
        o.call = rec.call
        o.need = False
        o.dma = dma
        o.sigval = None
        self.issue[e] += 1
        o.idx = self.issue[e]
        deps = {}

        def add(x):
            if x is None:
                return
            stream, idx, prod = x
            if stream == e:
                if e == 'pe' or (idx < o.idx - 1 and not STRICT_SYNC):
                    return
            if stream in deps and deps[stream][0] >= idx:
                return
            deps[stream] = (idx, prod)

        for k in r:
            add(self.lastw.get(k))
        for k in w:
            add(self.lastw.get(k))
            for x in self.readers.get(k, {}).values():
                add(x)
        o.deps = []
        for stream, (idx, prod) in deps.items():
            if self.waited[e].get(stream, 0) >= idx:
                continue
            self.waited[e][stream] = idx
            o.deps.append((stream, idx, prod))
            if prod is not None:
                prod.need = True
        if dma is not None:
            if dma not in self.sems:
                self.sems[dma] = self.es.enter_context(self.nc.semaphore("d_" + dma))
                self.dcnt[dma] = 0
            self.dcnt[dma] += 1
            me = (dma, self.dcnt[dma], None)
        else:
            me = (e, o.idx, o)
            self.lastop[e] = o
        for k in r:
            d = self.readers.setdefault(k, {})
            if me[0] not in d or d[me[0]][1] < me[1]:
                d[me[0]] = me
        for k in w:
            self.lastw[k] = me
            self.readers[k] = {}
        self.ops.append(o)

    def flush(self):
        for o in self.ops:
            if o.dma is None and o.need:
                self.sigcnt[o.e] += 1
                o.sigval = self.sigcnt[o.e]
        for o in self.ops:
            E = self.eng[o.e]
            for stream, idx, prod in o.deps:
                if prod is None:
                    E.wait_ge(self.sems[stream], 16 * idx)
                else:
                    E.wait_ge(self.sems[stream], prod.sigval)
            name, a, kw = o.call
            ins = getattr(E, name)(*a, **kw)
            if o.dma is not None:
                ins.then_inc(self.sems[o.dma], 16)
            elif o.need:
                ins.then_inc(self.sems[o.e], 1)
        self.ops = []

    def barrier(self):
        for e in self.COMPUTE:
            o = self.lastop.get(e)
            if o is not None and o.sigval is None:
                o.need = True
        self.flush()
        for e, E in self.eng.items():
            for s2 in self.COMPUTE:
                if self.sigcnt[s2] > 0 and (s2 != e or (STRICT_SYNC and e != 'pe')):
                    E.wait_ge(self.sems[s2], self.sigcnt[s2])
                self.waited[e][s2] = self.issue[s2]
            for d, c in self.dcnt.items():
                if c > 0:
                    E.wait_ge(self.sems[d], 16 * c)
                self.waited[e][d] = c
        self.lastw = {}
        self.readers = {}
        self.lastop = {}


def build_nc(NPRE=NPRE, NTL=NTL):
    nc = bass.Bass("TRN2", target_bir_lowering=False)

    def din(name, shape):
        return nc.dram_tensor(name, list(shape), F32, kind="ExternalInput").ap()

    xl = din("xl", [NPRE + NTL, 128, 8 * NT])
    winl = din("winl", [7, 128, 8192])
    woutl = din("woutl", [128, 8192])
    wql = din("wql", [4, 128, 4096])
    subkl = din("subkl", [128, 2048])
    ul = din("ul", [NJG, 128, 8 * JG * 128])
    vl = din("vl", [NJG, 128, JG * 1024])
    wal = din("wal", [128, 1024])
    wxl = din("wxl", [128, 1024])
    chpl = din("chpl", [128, 8 * NPARAM])
    identl = din("identl", [128, 128])
    iotal = din("iotal", [128, 128])
    onesl = din("onesl", [128, 128])
    hflagl = din("hflagl", [128, 1])
    outl = nc.dram_tensor("outl", [NTL, 128, 8 * NT], F32, kind="ExternalOutput").ap()
    win_bf = nc.dram_tensor("win_bf", [7, 128, 8192], BF16, kind="Internal").ap()
    wq_bf = nc.dram_tensor("wq_bf", [4, 128, 4096], BF16, kind="Internal").ap()
    u_bf = nc.dram_tensor("u_bf", [NJG, 128, 8 * JG * 128], BF16, kind="Internal").ap()
    v_bf = nc.dram_tensor("v_bf", [NJG, 128, JG * 1024], BF16, kind="Internal").ap()

    with ExitStack() as es:
        S = Sch(nc, es)

        uid = [0]

        def sb(stack, name, shape, dt):
            uid[0] += 1
            return stack.enter_context(nc.sbuf_tensor(f"{name}_{uid[0]}", list(shape), dt))

        def cap(t, off, dims):
            full = t[:]
            ps = full.ap[0][0]
            return bass.AP(full.tensor, off, [[ps, 128]] + [list(d) for d in dims])

        PS = [es.enter_context(nc.psum_tensor(f"bank{i}", [128, 512], F32)) for i in range(8)]
        wout = sb(es, "wout", [128, 8192], BF16)
        subk = sb(es, "subk", [128, 2048], BF16)
        wa = sb(es, "wa", [128, 1024], BF16)
        wx = sb(es, "wx", [128, 1024], BF16)
        ident = sb(es, "ident", [128, 128], F32)
        iota = sb(es, "iota", [128, 128], F32)
        ones = sb(es, "ones", [128, 128], F32)
        chp = sb(es, "chp", [128, 8 * NPARAM], F32)
        hflag = sb(es, "hflag", [128, 1], F32)
        c8 = sb(es, "c8", [128, 8], F32)
        tmp8 = sb(es, "tmp8", [128, 8], F32)
        xa_halo = sb(es, "xa_halo", [128, 8 * 3], F32)
        cv_halo = sb(es, "cv_halo", [128, 8 * 2], F32)
        hst = sb(es, "hst", [128, 8], F32)
        qT = sb(es, "qT", [128, 16 * NT], BF16)
        hT = [sb(es, f"hT{i}", [128, 8 * NT], F32) for i in range(2)]
        xn2 = [sb(es, f"xn2_{i}", [128, 8 * NT], BF16) for i in range(2)]
        Wt = sb(es, "Wt", [128, NT * 128], BF16)
        iT = sb(es, "iT", [128, NT], F32)
        jT = sb(es, "jT", [128, NT], F32)
        gT = sb(es, "gT", [128, NT], F32)
        sq = [sb(es, f"sq{i}", [128, NT], F32) for i in range(2)]
        rt = sb(es, "rt", [128, NT], F32)
        rstd = sb(es, "rstd", [128, NT], F32)
        c8h = sb(es, "c8h", [128, 8], F32)
        iota_bf = sb(es, "iota_bf", [128, 128], BF16)
        hba = sb(es, "hba", [128, 8], F32)
        hbx = sb(es, "hbx", [128, 8], F32)

        woutv = wout[:].rearrange("p (k n) -> p k n", k=8)
        subkv = subk[:].rearrange("p (h n) -> p h n", h=16)
        wav = wa[:].rearrange("p (h n) -> p h n", h=8)
        wxv = wx[:].rearrange("p (h n) -> p h n", h=8)
        chpv = chp[:].rearrange("p (c k) -> p c k", c=8)
        xav = xa_halo[:].rearrange("p (c k) -> p c k", c=8)
        cvv = cv_halo[:].rearrange("p (c k) -> p c k", c=8)
        qTv = qT[:].rearrange("p (h t) -> p h t", h=16)
        hTvs = [t[:].rearrange("p (c t) -> p c t", c=8) for t in hT]
        xn2v = [t[:].rearrange("p (c t) -> p c t", c=8) for t in xn2]

        def par(c, k):
            return chpv[:, c, k:k + 1]

        bank_rr = [0]

        def nb():
            bank_rr[0] = bank_rr[0] % 7 + 1
            return bank_rr[0]

        def act_copy(out, in_, r, w):
            S.op('act', lambda E: E.activation(out=out, in_=in_, func=AF.Identity), r=r, w=w)

        def gelu_from(src, srckeys, dst, dstkey, tmps, tmpkeys, post=None):
            if not GELU_MANUAL:
                fn = AF.Gelu_apprx_tanh if GELU_TANH else AF.Gelu
                if post is None:
                    S.op('act', lambda E: E.activation(out=dst, in_=src, func=fn), r=srckeys, w=[dstkey])
                else:
                    pap, pkey = post
                    S.op('act', lambda E: E.activation(out=tmps[0], in_=src, func=fn), r=srckeys, w=[tmpkeys[0]])
                    S.op('dve', lambda E: E.tensor_tensor(out=dst, in0=tmps[0], in1=pap, op=ALU.mult),
                         r=[tmpkeys[0], pkey], w=[dstkey])
                return
            t0, t1 = tmps
            k0, k1 = tmpkeys
            S.op('act', lambda E: E.activation(out=t0, in_=src, func=AF.Square), r=srckeys, w=[k0])
            S.op('pool', lambda E: E.tensor_scalar(out=t0, in0=t0, scalar1=0.044715, scalar2=1.0, op0=ALU.mult,
                                                   op1=ALU.add), r=[k0], w=[k0])
            S.op('dve', lambda E: E.tensor_tensor(out=t0, in0=src, in1=t0, op=ALU.mult), r=srckeys + [k0], w=[k0])
            S.op('act', lambda E: E.activation(out=t0, in_=t0, func=AF.Sigmoid, scale=1.5957691216057308),
                 r=[k0], w=[k0])
            if post is None:
                S.op('dve', lambda E: E.tensor_tensor(out=dst, in0=src, in1=t0, op=ALU.mult), r=srckeys + [k0], w=[dstkey])
            else:
                pap, pkey = post
                S.op('dve', lambda E: E.tensor_tensor(out=t1, in0=src, in1=t0, op=ALU.mult), r=srckeys + [k0], w=[k1])
                S.op('pool', lambda E: E.tensor_tensor(out=dst, in0=t1, in1=pap, op=ALU.mult), r=[k1, pkey], w=[dstkey])

        for (dst, src, key) in [(chp, chpl, 'chp'), (ident, identl, 'ident'), (iota, iotal, 'iota'),
                                (ones, onesl, 'ones'), (hflag, hflagl, 'hflag')]:
            S.op('sp', lambda E: E.dma_start(out=dst[:], in_=src[:, :]), w=[key], dma='cst_' + key)
        S.op('sp', lambda E: E.dma_start(out=hT[0][:], in_=xl[0]), w=['hT0'], dma='dx0')
        S.op('pool', lambda E: E.memset(xa_halo[:], 0.0), w=['xa_halo'])
        S.op('pool', lambda E: E.memset(cv_halo[:], 0.0), w=['cv_halo'])
        S.op('pool', lambda E: E.memset(hst[:], 0.0), w=['hst'])
        S.op('act', lambda E: E.activation(out=tmp8[:], in_=chpv[:, :, K_LAM], func=AF.Exp, scale=-1.0),
             r=['chp'], w=['tmp8'])
        S.op('act', lambda E: E.activation(out=tmp8[:], in_=tmp8[:], func=AF.Ln, bias=1.0, scale=1.0),
             r=['tmp8'], w=['tmp8'])
        S.op('dve', lambda E: E.tensor_copy(out=iota_bf[:], in_=iota[:]), r=['iota'], w=['iota_bf'])
        S.op('dve', lambda E: E.tensor_scalar(out=c8h[:], in0=tmp8[:], scalar1=-4.0, scalar2=None, op0=ALU.mult),
             r=['tmp8'], w=['c8h'])
        S.op('dve', lambda E: E.tensor_scalar(out=hba[:], in0=chpv[:, :, K_BA], scalar1=0.5, scalar2=None, op0=ALU.mult),
             r=['chp'], w=['hba'])
        S.op('dve', lambda E: E.tensor_scalar(out=hbx[:], in0=chpv[:, :, K_BX], scalar1=0.5, scalar2=None, op0=ALU.mult),
             r=['chp'], w=['hbx'])

        with ExitStack() as pst, nc.named_scope("prepass"):
            NST = 3
            st32 = [sb(pst, f"st32_{i}", [128, 4096], F32) for i in range(NST)]
            st16 = [sb(pst, f"st16_{i}", [128, 4096], BF16) for i in range(NST)]
            pieces = []
            pieces.append((woutl[:, 0:4096], None, (wout[:, 0:4096], 'wout'), 4096))
            pieces.append((woutl[:, 4096:8192], None, (wout[:, 4096:8192], 'wout'), 4096))
            pieces.append((subkl[:, :], None, (subk[:], 'subk'), 2048))
            pieces.append((wal[:, :], None, (wa[:], 'wa'), 1024))
            pieces.append((wxl[:, :], None, (wx[:], 'wx'), 1024))
            for g in range(7):
                for hh_ in range(2):
                    pieces.append((winl[g][:, hh_ * 4096:(hh_ + 1) * 4096], win_bf[g][:, hh_ * 4096:(hh_ + 1) * 4096],
                                   None, 4096))
            for q in range(4):
                pieces.append((wql[q], wq_bf[q], None, 4096))
            for g in range(NJG):
                pieces.append((ul[g], u_bf[g], None, 4096))
                pieces.append((vl[g], v_bf[g], None, 4096))
            cengs = ['dve', 'act']
            for i, (src, ddst, sdst, n) in enumerate(pieces):
                b = i % NST
                S.op('sp', lambda E: E.dma_start(out=st32[b][:, :n], in_=src), w=[f'st32_{b}'], dma=f'pin{b}')
                ce = cengs[i % 2]
                if sdst is not None:
                    o, okey = sdst
                else:
                    o, okey = st16[b][:, :n], f'st16_{b}'
                if ce == 'act':
                    S.op('act', lambda E: E.activation(out=o, in_=st32[b][:, :n], func=AF.Identity),
                         r=[f'st32_{b}'], w=[okey])
                else:
                    S.op(ce, lambda E: E.tensor_copy(out=o, in_=st32[b][:, :n]), r=[f'st32_{b}'], w=[okey])
                if ddst is not None:
                    S.op('sp', lambda E: E.dma_start(out=ddst, in_=st16[b][:, :n]), r=[okey], dma=f'pout{b}')
            S.barrier()

        def rmsnorm(srcv, skey, gk, dstv, dkey, slot=None, slotkey='b0'):
            if slot is None:
                slot = PS[0][:, :NT]
            for c in range(8):
                S.op('act', lambda E: E.activation(out=sq[c % 2][:], in_=srcv[:, c, :], func=AF.Square),
                     r=[skey], w=[f'sq{c % 2}'])
                S.op('pe', lambda E: E.matmul(slot, lhsT=ones[:], rhs=sq[c % 2][:],
                                              start=(c == 0), stop=(c == 7)),
                     r=['ones', f'sq{c % 2}'], w=[slotkey])
            S.op('act', lambda E: E.activation(out=rt[:], in_=slot, func=AF.Sqrt, bias=EPS, scale=1.0 / D),
                 r=[slotkey], w=['rt'])
            S.op('dve', lambda E: E.reciprocal(out=rstd[:], in_=rt[:]), r=['rt'], w=['rstd'])
            for c in range(8):
                S.op('dve', lambda E: E.scalar_tensor_tensor(out=dstv[:, c, :], in0=srcv[:, c, :], scalar=par(c, gk),
                                                             in1=rstd[:], op0=ALU.mult, op1=ALU.mult),
                     r=[skey, 'rstd', 'chp'], w=[dkey])

        def alloc_M(stk):
            M = {}
            M['win'] = [sb(stk, f"win{i}", [128, 8192], BF16) for i in range(2)]
            M['XA'] = sb(stk, "m_XA", [128, 8 * (NT + 4)], F32)
            M['CV'] = sb(stk, "m_CV", [128, 8 * (NT + 4)], F32)
            for nm in ['B2', 'B3', 'B4']:
                M[nm] = sb(stk, "m_" + nm, [128, 8 * NT], F32)
            M['xcb'] = sb(stk, "m_xcb", [128, 8 * NT], BF16)
            M['merged'] = M['xcb']
            M['wstate'] = {'g': [None, None], 'n': 0}
            return M

        def phase_M(ti, full, lastpre, M):
            hTv = hTvs[ti % 2]
            khT = f'hT{ti % 2}'
            xb = hTv
            kx = khT
            xnv = xn2v[ti % 2]
            kxn = f'xn2_{ti % 2}'

            def next_x():
                if ti + 1 < NPRE + NTL and ti <= NPRE:
                    nb_ = (ti + 1) % 2
                    S.op('sp', lambda E: E.dma_start(out=hT[nb_][:], in_=xl[ti + 1]), w=[f'hT{nb_}'], dma=f'dx{nb_}')
            needall = full or lastpre
            winv = [w_[:].rearrange("p (d n) -> p d n", d=8) for w_ in M['win']]
            ws = M['wstate']
            XA = M['XA'][:].rearrange("p (c t) -> p c t", c=8)
            CV = M['CV'][:].rearrange("p (c t) -> p c t", c=8)
            XC = M['B2'][:].rearrange("p (c t) -> p c t", c=8)
            RA = M['B3'][:].rearrange("p (c t) -> p c t", c=8)
            IG = M['B4'][:].rearrange("p (c t) -> p c t", c=8)
            T1 = XA[:, :, 0:NT]
            xcbv = M['xcb'][:].rearrange("p (c t) -> p c t", c=8)
            mergedv = M['merged'][:].rearrange("p (c t) -> p c t", c=8)
            glist = [0] + ([2, 4] if needall else []) + ([3, 1, 5, 6] if full else [])

            def ensure_w(g):
                for b in range(2):
                    if ws['g'][b] == g:
                        return b
                b = ws['n'] % 2
                ws['n'] += 1
                ws['g'][b] = g
                S.op('sp', lambda E: E.dma_start(out=M['win'][b][:], in_=win_bf[g]), w=[f'win{b}'], dma=f'dw{b}')
                return b

            def proj(g, c):
                b = ensure_w(g)
                bk = nb()
                for dc in range(8):
                    S.op('pe', lambda E: E.matmul(PS[bk][:, :NT], lhsT=winv[b][:, dc, c * 128:(c + 1) * 128],
                                                  rhs=xnv[:, dc, :], start=(dc == 0), stop=(dc == 7)),
                         r=[f'win{b}', kxn], w=[f'b{bk}'])
                return bk

            def stage_begin(g):
                ensure_w(g)
                i_ = glist.index(g)
                if i_ + 1 < len(glist):
                    ensure_w(glist[i_ + 1])

            ensure_w(glist[0])
            rmsnorm(xb, kx, K_G1, xnv, kxn)
            stage_begin(0)
            S.op('pool', lambda E: E.tensor_copy(out=XA[:, :, 0:3], in_=xav), r=['xa_halo'], w=['XA'])
            for c in range(8):
                bk = proj(0, c)
                act_copy(XA[:, c, 3:3 + NT], PS[bk][:, :NT], [f'b{bk}'], ['XA'])
            S.op('pool', lambda E: E.tensor_copy(out=xav, in_=XA[:, :, NT:NT + 3]), r=['XA'], w=['xa_halo'])
            for c in range(8):
                S.op('dve', lambda E: E.tensor_scalar(out=XC[:, c, :], in0=XA[:, c, 0:NT], scalar1=par(c, K_CAW),
                                                      scalar2=par(c, K_CAB), op0=ALU.mult, op1=ALU.add),
                     r=['XA', 'chp'], w=['B2'])
            for k in range(1, 4):
                for c in range(8):
                    S.op('dve', lambda E: E.scalar_tensor_tensor(out=XC[:, c, :], in0=XA[:, c, k:k + NT],
                                                                 scalar=par(c, K_CAW + k), in1=XC[:, c, :],
                                                                 op0=ALU.mult, op1=ALU.add),
                         r=['XA', 'chp', 'B2'], w=['B2'])
            act_copy(M['xcb'][:], M['B2'][:], ['B2'], ['xcb'])
            for c in range(8):
                b1 = nb()
                S.op('pe', lambda E: E.matmul(PS[b1][:, :NT], lhsT=wav[:, c, :], rhs=xcbv[:, c, :], start=True, stop=True),
                     r=['wa', 'xcb'], w=[f'b{b1}'])
                b2 = nb()
                S.op('pe', lambda E: E.matmul(PS[b2][:, :NT], lhsT=wxv[:, c, :], rhs=xcbv[:, c, :], start=True, stop=True),
                     r=['wx', 'xcb'], w=[f'b{b2}'])
                S.op('act', lambda E: E.activation(out=RA[:, c, :], in_=PS[b1][:, :NT], func=AF.Tanh,
                                                   bias=hba[:, c:c + 1], scale=0.5),
                     r=[f'b{b1}', 'hba'], w=['B3'])
                S.op('act', lambda E: E.activation(out=IG[:, c, :], in_=PS[b2][:, :NT], func=AF.Tanh,
                                                   bias=hbx[:, c:c + 1], scale=0.5),
                     r=[f'b{b2}', 'hbx'], w=['B4'])
            for c in range(8):
                S.op('act', lambda E: E.activation(out=RA[:, c, :], in_=RA[:, c, :], func=AF.Exp,
                                                   bias=c8h[:, c:c + 1], scale=c8h[:, c:c + 1]),
                     r=['B3', 'c8h'], w=['B3'])
            S.op('act', lambda E: E.activation(out=T1, in_=RA, func=AF.Square), r=['B3'], w=['XA'])
            S.op('dve', lambda E: E.tensor_scalar(out=T1, in0=T1, scalar1=1.0, scalar2=-1.0,
                                                  op0=ALU.min, op1=ALU.mult), r=['XA'], w=['XA'])
            S.op('dve', lambda E: E.tensor_scalar(out=M['B4'][:], in0=M['B4'][:], scalar1=0.5, scalar2=0.5,
                                                  op0=ALU.mult, op1=ALU.add), r=['B4'], w=['B4'])
            S.op('act', lambda E: E.activation(out=T1, in_=T1, func=AF.Sqrt, bias=1.0, scale=1.0),
                 r=['XA'], w=['XA'])
            S.op('dve', lambda E: E.tensor_tensor(out=M['B2'][:], in0=M['B2'][:], in1=M['B4'][:], op=ALU.mult),
                 r=['B2', 'B4'], w=['B2'])
            S.op('dve', lambda E: E.tensor_tensor(out=XC, in0=XC, in1=T1, op=ALU.mult),
                 r=['B2', 'XA'], w=['B2'])
            for c in range(8):
                S.op('dve', lambda E: E.tensor_tensor_scan(out=T1[:, c, :], data0=RA[:, c, :], data1=XC[:, c, :],
                                                           initial=hst[:, c:c + 1], op0=ALU.mult, op1=ALU.add),
                     r=['B3', 'B2', 'hst'], w=['XA'])
            if lastpre:
                S.op('pool', lambda E: E.tensor_scalar(out=hst[:], in0=T1[:, :, NT - 1], scalar1=hflag[:, 0:1],
                                                       scalar2=None, op0=ALU.mult),
                     r=['XA', 'hflag'], w=['hst'])
            else:
                S.op('pool', lambda E: E.tensor_copy(out=hst[:], in_=T1[:, :, NT - 1]), r=['XA'], w=['hst'])
            if not needall:
                next_x()
                return
            VB = XC
            stage_begin(2)
            for c in range(8):
                bk = proj(2, c)
                act_copy(VB[:, c, :], PS[bk][:, :NT], [f'b{bk}'], ['B2'])
            S.op('pool', lambda E: E.tensor_copy(out=CV[:, :, 0:2], in_=cvv), r=['cv_halo'], w=['CV'])
            stage_begin(4)
            for c in range(8):
                bk = proj(4, c)
                S.op('dve', lambda E: E.tensor_tensor(out=CV[:, c, 2:2 + NT], in0=PS[bk][:, :NT], in1=VB[:, c, :],
                                                      op=ALU.mult),
                     r=[f'b{bk}', 'B2'], w=['CV'])
            S.op('pool', lambda E: E.tensor_copy(out=cvv, in_=CV[:, :, NT:NT + 2]), r=['CV'], w=['cv_halo'])
            if not full:
                next_x()
                return
            YCV = RA
            for c in range(8):
                S.op('dve', lambda E: E.tensor_scalar(out=YCV[:, c, :], in0=CV[:, c, 0:NT], scalar1=par(c, K_CBW),
                                                      scalar2=None, op0=ALU.mult),
                     r=['CV', 'chp'], w=['B3'])
            for k in range(1, 3):
                for c in range(8):
                    S.op('dve', lambda E: E.scalar_tensor_tensor(out=YCV[:, c, :], in0=CV[:, c, k:k + NT],
                                                                 scalar=par(c, K_CBW + k), in1=YCV[:, c, :],
                                                                 op0=ALU.mult, op1=ALU.add),
                         r=['CV', 'chp', 'B3'], w=['B3'])
            stage_begin(3)
            for c in range(8):
                bk = proj(3, c)
                S.op('dve', lambda E: E.tensor_tensor(out=YCV[:, c, :], in0=PS[bk][:, :NT], in1=YCV[:, c, :], op=ALU.mult),
                     r=[f'b{bk}', 'B3'], w=['B3'])
            GG = IG
            stage_begin(1)
            for c in range(8):
                bk = proj(1, c)
                S.op('act', lambda E: E.activation(out=GG[:, c, :], in_=PS[bk][:, :NT], func=AF.Gelu_apprx_tanh),
                     r=[f'b{bk}'], w=['B4'])
            SA = CV
            SB = XC
            stage_begin(5)
            for c in range(8):
                bk = proj(5, c)
                S.op('act', lambda E: E.activation(out=SA[:, c, 0:NT], in_=PS[bk][:, :NT], func=AF.Tanh, scale=0.5),
                     r=[f'b{bk}'], w=['CV'])
            stage_begin(6)
            for c in range(8):
                bk = proj(6, c)
                S.op('act', lambda E: E.activation(out=SB[:, c, :], in_=PS[bk][:, :NT], func=AF.Tanh, scale=0.5),
                     r=[f'b{bk}'], w=['B2'])
            S.op('dve', lambda E: E.tensor_tensor(out=GG, in0=GG, in1=T1, op=ALU.mult), r=['B4', 'XA'], w=['B4'])
            S.op('dve', lambda E: E.tensor_scalar(out=SA[:, :, 0:NT], in0=SA[:, :, 0:NT], scalar1=0.5, scalar2=0.5,
                                                  op0=ALU.mult, op1=ALU.add), r=['CV'], w=['CV'])
            S.op('dve', lambda E: E.tensor_scalar(out=SB, in0=SB, scalar1=0.5, scalar2=0.5,
                                                  op0=ALU.mult, op1=ALU.add), r=['B2'], w=['B2'])
            S.op('dve', lambda E: E.tensor_tensor(out=GG, in0=GG, in1=SA[:, :, 0:NT], op=ALU.mult),
                 r=['B4', 'CV'], w=['B4'])
            S.op('dve', lambda E: E.tensor_tensor(out=YCV, in0=YCV, in1=SB, op=ALU.mult), r=['B3', 'B2'], w=['B3'])
            S.op('dve', lambda E: E.tensor_tensor(out=mergedv, in0=GG, in1=YCV, op=ALU.add),
                 r=['B4', 'B3'], w=['xcb'])
            for oc in range(8):
                bk = nb()
                for kc in range(8):
                    S.op('pe', lambda E: E.matmul(PS[bk][:, :NT], lhsT=woutv[:, kc, oc * 128:(oc + 1) * 128],
                                                  rhs=mergedv[:, kc, :], start=(kc == 0), stop=(kc == 7)),
                         r=['wout', 'xcb'], w=[f'b{bk}'])
                S.op('dve', lambda E: E.tensor_tensor(out=hTv[:, oc, :], in0=PS[bk][:, :NT], in1=hTv[:, oc, :], op=ALU.add),
                     r=[f'b{bk}', khT], w=[khT])
            next_x()
            for piece in range(4):
                b = piece // 2
                lo = (piece % 2) * 4096
                S.op('sp', lambda E: E.dma_start(out=M['win'][b][:, lo:lo + 4096], in_=wq_bf[piece]),
                     w=[f'win{b}'], dma=f'dw{b}')
            rmsnorm(hTv, khT, K_G2, xnv, kxn)
            for hp in range(16):
                piece, k = hp // 4, hp % 4
                b = piece // 2
                wq_ = M['win'][b][:, (piece % 2) * 4096 + k * 1024:(piece % 2) * 4096 + (k + 1) * 1024]
                wqv_ = wq_.rearrange("p (d n) -> p d n", d=8)
                bk = nb()
                for dc in range(8):
                    S.op('pe', lambda E: E.matmul(PS[bk][:, :NT], lhsT=wqv_[:, dc, :], rhs=xnv[:, dc, :],
                                                  start=(dc == 0), stop=(dc == 7)),
                         r=[f'win{b}', kxn], w=[f'b{bk}'])
                act_copy(qTv[:, hp, :], PS[bk][:, :NT], [f'b{bk}'], ['qT'])

        def alloc_P1a(stk):
            T = {}
            T['sc'] = sb(stk, "sc", [128, 2048], F32)
            T['m'] = sb(stk, "m", [128, 256], F32)
            T['iu'] = sb(stk, "iu", [128, 256], U32)
            T['idf'] = sb(stk, "idf", [128, 256], F32)
            T['wk1s'] = [sb(stk, f"wk1_{i}", [128, 128], F32) for i in range(2)]
            T['cand'] = sb(stk, "cand", [128, 2048], F32)
            T['wk2s'] = [sb(stk, f"wk2_{i}", [128, 256], F32) for i in range(2)]
            for nm, dt_ in [('c16', F32), ('pu', U32), ('au', U32), ('bu', U32), ('af', F32), ('bf_', F32),
                            ('e16', F32), ('gsel', F32), ('isel', F32), ('jsel', F32)]:
                T[nm] = sb(stk, nm, [128, 128], dt_)
            for nm in ['negmax', 'Z', 'rz']:
                T[nm] = sb(stk, nm, [128, 8], F32)
            return T

        def gen_P1a(ti, T, slots):
            par_ = ti % 2
            hTv = hTvs[par_]
            khT = f'hT{par_}'
            x2v = xn2v[par_]
            kx2 = f'xn2_{par_}'
            (nslot, nkey), xs = slots
            xi = [0]

            def nslot_():
                xi[0] = (xi[0] + 1) % len(xs)
                return xs[xi[0]]

            sc, m, iu, idf = T['sc'], T['m'], T['iu'], T['idf']
            wk1s, cand, wk2s = T['wk1s'], T['cand'], T['wk2s']
            c16, pu, au, bu, af, bf_ = T['c16'], T['pu'], T['au'], T['bu'], T['af'], T['bf_']
            negmax, Z, rz, e16, gsel, isel, jsel = T['negmax'], T['Z'], T['rz'], T['e16'], T['gsel'], T['isel'], T['jsel']
            scv = sc[:].rearrange("p (h n) -> p h n", h=16)
            mv = m[:].rearrange("p (h k) -> p h k", h=16)
            iuv = iu[:].rearrange("p (h k) -> p h k", h=16)
            candv = cand[:].rearrange("p (h a b) -> p h a b", h=8, a=16)
            cand3 = cand[:].rearrange("p (h ab) -> p h ab", h=8)
            c16v = c16[:].rearrange("p (h k) -> p h k", h=8)
            puv = pu[:].rearrange("p (h k) -> p h k", h=8)
            e16v = e16[:].rearrange("p (h k) -> p h k", h=8)
            gselv = gsel[:].rearrange("p (h k) -> p h k", h=8)
            PU = [f'pu{h}' for h in range(8)]
            C16 = [f'c16_{h}' for h in range(8)]

            for s in range(NT // 128):
                for hp4 in range(4):
                    for k2 in range(2):
                        sl, skeys = nslot_()
                        for k in range(2):
                            hp = hp4 * 4 + k2 * 2 + k
                            S.op('pe', lambda E: E.matmul(sl(k * 128, 128), lhsT=qTv[:, hp, s * 128:(s + 1) * 128],
                                                          rhs=subkv[:, hp, :], start=True, stop=True),
                                 r=['qT', 'subk'], w=skeys)
                        c0 = (hp4 * 4 + k2 * 2) * 128
                        act_copy(sc[:, c0:c0 + 256], sl(0, 256), skeys, ['sc'])
                    yield
                for hp0 in range(0, 16, 2):
                    pr = [(hp0, wk1s[0], 'wk1_0'), (hp0 + 1, wk1s[1], 'wk1_1')]
                    for hp, wk1, kk in pr:
                        S.op('dve', lambda E: E.max(out=mv[:, hp, 0:8], in_=scv[:, hp, :]), r=['sc'], w=[f'm{hp}'])
                    for hp, wk1, kk in pr:
                        S.op('dve', lambda E: E.max_index(out=iuv[:, hp, 0:8], in_max=mv[:, hp, 0:8],
                                                          in_values=scv[:, hp, :]), r=['sc', f'm{hp}'], w=[f'iu{hp}'])
                    for hp, wk1, kk in pr:
                        S.op('dve', lambda E: E.match_replace(out=wk1[:], in_to_replace=mv[:, hp, 0:8],
                                                              in_values=scv[:, hp, :], imm_value=-1e30),
                             r=['sc', f'm{hp}'], w=[kk])
                    for hp, wk1, kk in pr:
                        S.op('dve', lambda E: E.max(out=mv[:, hp, 8:16], in_=wk1[:]), r=[kk], w=[f'm{hp}'])
                    for hp, wk1, kk in pr:
                        S.op('dve', lambda E: E.max_index(out=iuv[:, hp, 8:16], in_max=mv[:, hp, 8:16], in_values=wk1[:]),
                             r=[kk, f'm{hp}'], w=[f'iu{hp}'])
                    yield
                S.op('dve', lambda E: E.tensor_copy(out=idf[:], in_=iu[:]), r=[f'iu{q}' for q in range(16)], w=['idf'])
                S.op('dve', lambda E: E.tensor_tensor(out=candv, in0=cap(m, 0, [[32, 8], [1, 16], [0, 16]]),
                                                      in1=cap(m, 16, [[32, 8], [0, 16], [1, 16]]), op=ALU.add),
                     r=[f'm{q}' for q in range(16)], w=['cand'])
                yield
                for h0 in range(0, 8, 2):
                    pr = [(h0, wk2s[0], 'wk2_0'), (h0 + 1, wk2s[1], 'wk2_1')]
                    for h, wk2, kk in pr:
                        S.op('dve', lambda E: E.max(out=c16v[:, h, 0:8], in_=cand3[:, h, :]), r=['cand'], w=[f'c16_{h}'])
                    for h, wk2, kk in pr:
                        S.op('dve', lambda E: E.max_index(out=puv[:, h, 0:8], in_max=c16v[:, h, 0:8],
                                                          in_values=cand3[:, h, :]), r=['cand', f'c16_{h}'], w=[f'pu{h}'])
                    for h, wk2, kk in pr:
                        S.op('dve', lambda E: E.match_replace(out=wk2[:], in_to_replace=c16v[:, h, 0:8],
                                                              in_values=cand3[:, h, :], imm_value=-1e30),
                             r=['cand', f'c16_{h}'], w=[kk])
                    for h, wk2, kk in pr:
                        S.op('dve', lambda E: E.max(out=c16v[:, h, 8:16], in_=wk2[:]), r=[kk], w=[f'c16_{h}'])
                    for h, wk2, kk in pr:
                        S.op('dve', lambda E: E.max_index(out=puv[:, h, 8:16], in_max=c16v[:, h, 8:16], in_values=wk2[:]),
                             r=[kk, f'c16_{h}'], w=[f'pu{h}'])
                    yield
                S.op('dve', lambda E: E.tensor_single_scalar(out=au[:], in_=pu[:], scalar=4, op=ALU.logical_shift_right),
                     r=PU, w=['au'])
                S.op('dve', lambda E: E.tensor_single_scalar(out=bu[:], in_=pu[:], scalar=15, op=ALU.bitwise_and),
                     r=PU, w=['bu'])
                S.op('dve', lambda E: E.tensor_copy(out=af[:], in_=au[:]), r=['au'], w=['af'])
                S.op('dve', lambda E: E.tensor_copy(out=bf_[:], in_=bu[:]), r=['bu'], w=['bf_'])
                S.op('dve', lambda E: E.tensor_scalar(out=negmax[:], in0=c16v[:, :, 0], scalar1=-1.0, scalar2=None,
                                                      op0=ALU.mult), r=C16, w=['negmax'])
                for h in range(8):
                    S.op('act', lambda E: E.activation(out=e16v[:, h, :], in_=c16v[:, h, :], func=AF.Exp,
                                                       bias=negmax[:, h:h + 1], scale=1.0),
                         r=[f'c16_{h}', 'negmax'], w=['e16'])
                yield
                S.op('dve', lambda E: E.tensor_reduce(out=Z[:], in_=e16v, axis=AX.X, op=ALU.add), r=['e16'], w=['Z'])
                S.op('dve', lambda E: E.reciprocal(out=rz[:], in_=Z[:]), r=['Z'], w=['rz'])
                S.op('dve', lambda E: E.tensor_tensor(out=gselv, in0=e16v, in1=cap(rz, 0, [[1, 8], [0, 16]]), op=ALU.mult),
                     r=['e16', 'rz'], w=['gsel'])
                oh3 = cand[:].rearrange("p (k a) -> p k a", a=16)
                oh4 = cand[:].rearrange("p (h k a) -> p h k a", h=8, k=16)
                for (sel, selkey, abf, abkey, off) in [(isel, 'isel', af, 'af', 0), (jsel, 'jsel', bf_, 'bf_', 16)]:
                    S.op('dve', lambda E: E.tensor_tensor(out=oh3, in0=cap(iota, 0, [[0, 128], [1, 16]]),
                                                          in1=cap(abf, 0, [[1, 128], [0, 16]]), op=ALU.is_equal),
                         r=['iota', abkey], w=['cand'])
                    yield
                    S.op('dve', lambda E: E.tensor_tensor(out=oh4, in0=oh4, in1=cap(idf, off, [[32, 8], [0, 16], [1, 16]]),
                                                          op=ALU.mult),
                         r=['cand', 'idf'], w=['cand'])
                    S.op('dve', lambda E: E.tensor_reduce(out=sel[:], in_=oh3, axis=AX.X, op=ALU.add),
                         r=['cand'], w=[selkey])
                    yield
                for (src, skey, dstT, dkey) in [(isel, 'isel', iT, 'iT'), (jsel, 'jsel', jT, 'jT'), (gsel, 'gsel', gT, 'gT')]:
                    sl, skeys = nslot_()
                    S.op('pe', lambda E: E.transpose(out=sl(0, 128), in_=src[:], identity=ident[:]),
                         r=[skey, 'ident'], w=skeys)
                    act_copy(dstT[:, s * 128:(s + 1) * 128], sl(0, 128), skeys, [dkey])
                yield

        def mkslot(bank, half=0):
            return (lambda off, n: PS[bank][:, off: off + n]), [f'b{bank}']

        def phase_WB(ti, stk):
            OH1 = [sb(stk, f"OH1_{i}", [128, TB * 128], BF16) for i in range(2)]
            OH2 = [sb(stk, f"OH2_{i}", [128, TB * 128], BF16) for i in range(2)]
            wbanks = [1, 2, 3, 4]
            for tb in range(NT // TB):
                t0 = tb * TB
                ob = tb % 2
                o1 = OH1[ob][:].rearrange("p (t i) -> p t i", t=TB)
                o2 = OH2[ob][:].rearrange("p (t i) -> p t i", t=TB)
                k1, k2 = f'OH1_{ob}', f'OH2_{ob}'
                S.op('dve', lambda E: E.tensor_tensor(out=o2, in0=cap(iota_bf, 0, [[0, TB], [1, 128]]),
                                                      in1=cap(jT, t0, [[1, TB], [0, 128]]), op=ALU.is_equal),
                     r=['iota_bf', 'jT'], w=[k2])
                for tt in range(TB):
                    t = t0 + tt
                    S.op('dve', lambda E: E.tensor_scalar(out=o1[:, tt, :], in0=iota_bf[:], scalar1=iT[:, t:t + 1],
                                                          scalar2=gT[:, t:t + 1], op0=ALU.is_equal, op1=ALU.mult),
                         r=['iota_bf', 'iT', 'gT'], w=[k1])
                for tt in range(TB):
                    t = t0 + tt
                    bk = wbanks[(t // 4) % 4]
                    S.op('pe', lambda E: E.matmul(PS[bk][:, (t % 4) * 128:(t % 4 + 1) * 128], lhsT=o1[:, tt, :],
                                                  rhs=o2[:, tt, :], start=True, stop=True),
                         r=[k1, k2], w=[f'b{bk}'])
                    if t % 4 == 3:
                        act_copy(Wt[:, (t - 3) * 128:(t + 1) * 128], PS[bk][:, :512], [f'b{bk}'], ['Wt'])

        def phase_P2(ti, stk, gen):
            par_ = ti % 2
            hTv = hTvs[par_]
            khT = f'hT{par_}'
            x2v = xn2v[par_]
            kx2 = f'xn2_{par_}'
            U = [sb(stk, f"U{i}", [128, 8 * JG * 128], BF16) for i in range(2)]
            Vv = [sb(stk, f"V{i}", [128, JG * 1024], BF16) for i in range(2)]
            gt0 = [sb(stk, f"gt0_{i}", [128, NT], F32) for i in range(2)]
            Cb = [sb(stk, f"C{i}", [128, JG * NT], BF16) for i in range(2)]
            Uv = [u_[:].rearrange("p (d j i) -> p d j i", d=8, j=JG) for u_ in U]
            Vvv = [v_[:].rearrange("p (j d) -> p j d", j=JG) for v_ in Vv]
            Cv = [c_[:].rearrange("p (j t) -> p j t", j=JG) for c_ in Cb]
            aslots = [mkslot(0), mkslot(1)]

            def load(g):
                b = g % 2
                S.op('sp', lambda E: E.dma_start(out=U[b][:], in_=u_bf[g]), w=[f'U{b}'], dma=f'du{b}')
                S.op('sp', lambda E: E.dma_start(out=Vv[b][:], in_=v_bf[g]), w=[f'V{b}'], dma=f'dv{b}')

            def pull(n):
                if gen is None:
                    return
                for _ in range(n):
                    try:
                        next(gen)
                    except StopIteration:
                        return

            if NO_INTERLEAVE:
                pull(10000)
            else:
                pull(PRE_PULL)
            def loadU(g):
                b = g % 2
                S.op('sp', lambda E: E.dma_start(out=U[b][:], in_=u_bf[g]), w=[f'U{b}'], dma=f'du{b}')

            def loadV(g):
                b = g % 2
                S.op('sp', lambda E: E.dma_start(out=Vv[b][:], in_=v_bf[g]), w=[f'V{b}'], dma=f'dv{b}')

            def vside(g, dcs):
                b = g % 2
                for dc in dcs:
                    bk = 4 + dc // 2
                    lo = (dc % 2) * NT
                    for jj in range(JG):
                        S.op('pe', lambda E: E.matmul(PS[bk][:, lo:lo + NT], lhsT=Vvv[b][:, jj, dc * 128:(dc + 1) * 128],
                                                      rhs=Cv[b][:, jj, :],
                                                      start=(g == 0 and jj == 0 and dc % 2 == 0),
                                                      stop=(g == NJG - 1 and jj == JG - 1),
                                                      skip_group_check=True),
                             r=[f'V{b}', f'C{b}'], w=[f'pb{4 + dc // 2}'])

            loadU(0)
            loadV(0)
            loadV(1)
            na = 0
            for jg in range(NJG):
                if jg + 1 < NJG:
                    loadU(jg + 1)
                b = jg % 2
                for jj in range(JG):
                    j = jg * JG + jj
                    sl, skeys = aslots[na % 2]
                    na += 1
                    for dc in range(8):
                        S.op('pe', lambda E: E.matmul(sl(0, NT), lhsT=Uv[b][:, dc, jj, :], rhs=x2v[:, dc, :],
                                                      start=(dc == 0), stop=(dc == 7)),
                             r=[f'U{b}', kx2], w=skeys)
                    gb = jj % 2
                    S.op('act', lambda E: E.activation(out=gt0[gb][:], in_=sl(0, NT), func=AF.Gelu_apprx_tanh),
                         r=skeys, w=[f'gt0_{gb}'])
                    S.op('dve', lambda E: E.tensor_tensor(out=Cv[b][:, jj, :], in0=gt0[gb][:],
                                                          in1=cap(Wt, j, [[128, NT]]), op=ALU.mult),
                         r=[f'gt0_{gb}', 'Wt'], w=[f'C{b}'])
                    if jg > 0:
                        vside(jg - 1, [2 * jj, 2 * jj + 1])
                if jg > 0 and jg + 1 < NJG:
                    loadV(jg + 1)
                pull(2)
            vside(NJG - 1, list(range(8)))
            pull(10000)
            for dc in range(8):
                bk = 4 + dc // 2
                lo = (dc % 2) * NT
                S.op('dve', lambda E: E.tensor_tensor(out=hTv[:, dc, :], in0=PS[bk][:, lo:lo + NT], in1=hTv[:, dc, :],
                                                      op=ALU.add),
                     r=[f'pb{4 + dc // 2}', khT], w=[khT])
            rmsnorm(hTv, khT, K_GF, hTv, khT, slot=PS[2][:, :NT], slotkey='b2')
            S.op('sp', lambda E: E.dma_start(out=outl[ti - NPRE], in_=hT[par_][:]), r=[khT], dma='dout')
            if ti + 2 < NPRE + NTL:
                S.op('sp', lambda E: E.dma_start(out=hT[par_][:], in_=xl[ti + 2]), w=[khT], dma=f'dx{par_}')

        p1slots = ((PS[2][:, :NT], 'b2'), [mkslot(3)])
        with ExitStack() as stk:
            M = alloc_M(stk)
            for ti in range(NPRE):
                with nc.named_scope(f"pre{ti}"):
                    phase_M(ti, False, ti == NPRE - 1, M)
            S.barrier()
        t_first, t_last = NPRE, NPRE + NTL - 1
        with ExitStack() as stk:
            M = alloc_M(stk)
            with nc.named_scope(f"M{t_first}"):
                phase_M(t_first, True, False, M)
            S.barrier()
        with ExitStack() as stk:
            T = alloc_P1a(stk)
            with nc.named_scope(f"P1a_{t_first}"):
                for _ in gen_P1a(t_first, T, p1slots):
                    pass
            S.barrier()
        with ExitStack() as stk:
            with nc.named_scope(f"WB_{t_first}"):
                phase_WB(t_first, stk)
            S.barrier()
        for ti in range(t_first, t_last + 1):
            more = ti + 1 <= t_last
            if more:
                with ExitStack() as stk:
                    M = alloc_M(stk)
                    with nc.named_scope(f"M{ti + 1}"):
                        phase_M(ti + 1, True, False, M)
                    S.barrier()
            with ExitStack() as stk:
                gen = None
                if more:
                    T = alloc_P1a(stk)
                    gen = gen_P1a(ti + 1, T, p1slots)
                with nc.named_scope(f"OV_{ti}"):
                    phase_P2(ti, stk, gen)
                S.barrier()
            if more:
                with ExitStack() as stk:
                    with nc.named_scope(f"WB_{ti + 1}"):
                        phase_WB(ti + 1, stk)
                    S.barrier()
        nc.sync.wait_ge(S.sems['dout'], 16 * S.dcnt['dout'])
    return nc


_NC_CACHE = {}


def _layouts(x, norm1_g, w_in, conv_a_w, conv_a_b, w_a, b_a, w_x, b_x, lru_lambda, conv_b_w, w_out,
             norm2_g, peer_wq, peer_subkeys, peer_u, peer_v, final_g):
    f = np.float32
    shared = {}
    w = np.asarray(w_in[0], f).reshape(8, 128, 7, 1024)
    shared["winl"] = np.ascontiguousarray(w.transpose(2, 1, 0, 3)).reshape(7, 128, 8192)
    shared["woutl"] = np.ascontiguousarray(np.asarray(w_out[0], f).reshape(8, 128, 1024).transpose(1, 0, 2)).reshape(128, 8192)
    wq = np.asarray(peer_wq[0], f).reshape(8, 128, 4, 4, 128)
    shared["wql"] = np.ascontiguousarray(wq.transpose(2, 1, 3, 0, 4)).reshape(4, 128, 4096)
    sk = np.asarray(peer_subkeys[0], f).reshape(16, 128, 128)
    shared["subkl"] = np.ascontiguousarray(sk.transpose(2, 0, 1)).reshape(128, 2048)
    u = np.asarray(peer_u[0], f).reshape(128, NJG, JG, 8, 128)
    shared["ul"] = np.ascontiguousarray(u.transpose(1, 4, 3, 2, 0)).reshape(NJG, 128, 8 * JG * 128)
    v = np.asarray(peer_v[0], f).reshape(128, NJG, JG, 1024)
    shared["vl"] = np.ascontiguousarray(v.transpose(1, 0, 2, 3)).reshape(NJG, 128, JG * 1024)
    shared["wal"] = np.ascontiguousarray(np.asarray(w_a[0], f).transpose(1, 0, 2)).reshape(128, 1024)
    shared["wxl"] = np.ascontiguousarray(np.asarray(w_x[0], f).transpose(1, 0, 2)).reshape(128, 1024)
    chp = np.zeros((128, 8, NPARAM), f)

    def pc(vec):
        return np.asarray(vec, f).reshape(8, 128).T

    chp[:, :, K_G1] = pc(norm1_g[0])
    for k in range(4):
        chp[:, :, K_CAW + k] = pc(conv_a_w[0][k])
    chp[:, :, K_CAB] = pc(conv_a_b[0])
    chp[:, :, K_BA] = pc(b_a[0])
    chp[:, :, K_BX] = pc(b_x[0])
    chp[:, :, K_LAM] = pc(lru_lambda[0])
    for k in range(3):
        chp[:, :, K_CBW + k] = pc(conv_b_w[0][k])
    chp[:, :, K_G2] = pc(norm2_g[0])
    chp[:, :, K_GF] = pc(final_g)
    shared["chpl"] = chp.reshape(128, 8 * NPARAM)
    shared["identl"] = np.eye(128, dtype=f)
    shared["iotal"] = np.ascontiguousarray(np.broadcast_to(np.arange(128, dtype=f)[None, :], (128, 128)))
    shared["onesl"] = np.ones((128, 128), f)
    return shared


def kernel(x, norm1_g, w_in, conv_a_w, conv_a_b, w_a, b_a, w_x, b_x, lru_lambda, conv_b_w, w_out,
           norm2_g, peer_wq, peer_subkeys, peer_u, peer_v, final_g):
    f = np.float32
    x = np.asarray(x, f)
    B, Sq, _ = x.shape
    shared = _layouts(x, norm1_g, w_in, conv_a_w, conv_a_b, w_a, b_a, w_x, b_x, lru_lambda, conv_b_w, w_out,
                      norm2_g, peer_wq, peer_subkeys, peer_u, peer_v, final_g)
    in_maps = []
    for core in range(8):
        b, half = core // 2, core % 2
        win = np.zeros((2 * TOK, D), f)
        win[TOK:] = x[b, half * TOK:(half + 1) * TOK]
        if half == 1:
            win[:TOK] = x[b, 0:TOK]
        xl = np.ascontiguousarray(win.reshape(NPRE + NTL, NT, 8, 128).transpose(0, 3, 2, 1)).reshape(NPRE + NTL, 128, 8 * NT)
        m = dict(shared)
        m["xl"] = xl
        m["hflagl"] = np.full((128, 1), float(half), f)
        in_maps.append(m)
    if "nc" not in _NC_CACHE:
        _NC_CACHE["nc"] = build_nc()
    nc = _NC_CACHE["nc"]
    res = run_bass_kernel_spmd(nc, in_maps, core_ids=list(range(8)))
    out = np.zeros((B, Sq, D), f)
    for core in range(8):
        b, half = core // 2, core % 2
        o = np.asarray(res.results[core]["outl"], f).reshape(NTL, 128, 8, NT)
        o = o.transpose(0, 3, 2, 1).reshape(TOK, D)
        out[b, half * TOK:(half + 1) * TOK] = o
    return out
```

```python
import numpy as np
from contextlib import ExitStack
import concourse.bass as bass
import concourse.mybir as mybir
from concourse.bass_utils import run_bass_kernel_spmd

F32 = mybir.dt.float32
BF16 = mybir.dt.bfloat16
U32 = mybir.dt.uint32
AF = mybir.ActivationFunctionType
ALU = mybir.AluOpType
AX = mybir.AxisListType

D = 1024
NT = 256
TOK = 4096
NTL = TOK // NT
NPRE = TOK // NT
JG = 4
NJG = 128 // JG
TB = 8
EPS = 1e-6
GELU_TANH = True
GELU_MANUAL = False
NO_INTERLEAVE = False
PRE_PULL = 0
STRICT_SYNC = False

K_G1, K_CAW, K_CAB, K_BA, K_BX, K_LAM, K_CBW, K_G2, K_GF, NPARAM = 0, 1, 5, 6, 7, 8, 9, 12, 13, 14


class _Rec:
    def __init__(self):
        self.call = None

    def __getattr__(self, name):
        def f(*a, **kw):
            self.call = (name, a, kw)
            return self
        return f


class _Op:
    __slots__ = ("e", "call", "deps", "need", "dma", "idx", "sigval")


class Sch:
    COMPUTE = ('pe', 'act', 'dve', 'pool')

    def __init__(self, nc, es):
        self.nc = nc
        self.es = es
        self.eng = {'pe': nc.tensor, 'act': nc.scalar, 'dve': nc.vector, 'pool': nc.gpsimd, 'sp': nc.sync}
        self.sems = {}
        self.sigcnt = {}
        self.issue = {}
        for e in self.eng:
            self.sems[e] = es.enter_context(nc.semaphore("s_" + e))
            self.sigcnt[e] = 0
            self.issue[e] = 0
        self.dcnt = {}
        self.waited = {e: {} for e in self.eng}
        self.lastw = {}
        self.readers = {}
        self.ops = []
        self.lastop = {}

    def op(self, e, fn, r=(), w=(), dma=None, sig=True):
        rec = _Rec()
        fn(rec)
        o = _Op()
        o.e = e
        o.call = rec.call
        o.need = False
        o.dma = dma
        o.sigval = None
        self.issue[e] += 1
        o.idx = self.issue[e]
        deps = {}

        def add(x):
            if x is None:
                return
            stream, idx, prod = x
            if stream == e:
                if e == 'pe' or (idx < o.idx - 1 and not STRICT_SYNC):
                    return
            if stream in deps and deps[stream][0] >= idx:
                return
            deps[stream] = (idx, prod)

        for k in r:
            add(self.lastw.get(k))
        for k in w:
            add(self.lastw.get(k))
            for x in self.readers.get(k, {}).values():
                add(x)
        o.deps = []
        for stream, (idx, prod) in deps.items():
            if self.waited[e].get(stream, 0) >= idx:
                continue
            self.waited[e][stream] = idx
            o.deps.append((stream, idx, prod))
            if prod is not None:
                prod.need = True
        if dma is not None:
            if dma not in self.sems:
                self.sems[dma] = self.es.enter_context(self.nc.semaphore("d_" + dma))
                self.dcnt[dma] = 0
            self.dcnt[dma] += 1
            me = (dma, self.dcnt[dma], None)
        else:
            me = (e, o.idx, o)
            self.lastop[e] = o
        for k in r:
            d = self.readers.setdefault(k, {})
            if me[0] not in d or d[me[0]][1] < me[1]:
                d[me[0]] = me
        for k in w:
            self.lastw[k] = me
            self.readers[k] = {}
        self.ops.append(o)

    def flush(self):
        for o in self.ops:
            if o.dma is None and o.need:
                self.sigcnt[o.e] += 1
                o.sigval = self.sigcnt[o.e]
        for o in self.ops:
            E = self.eng[o.e]
            for stream, idx, prod in o.deps:
                if prod is None:
                    E.wait_ge(self.sems[stream], 16 * idx)
                else:
                    E.wait_ge(self.sems[stream], prod.sigval)
            name, a, kw = o.call
            ins = getattr(E, name)(*a, **kw)
            if o.dma is not None:
                ins.then_inc(self.sems[o.dma], 16)
            elif o.need:
                ins.then_inc(self.sems[o.e], 1)
        self.ops = []

    def barrier(self):
        for e in self.COMPUTE:
            o = self.lastop.get(e)
            if o is not None and o.sigval is None:
                o.need = True
        self.flush()
        for e, E in self.eng.items():
            for s2 in self.COMPUTE:
                if self.sigcnt[s2] > 0 and (s2 != e or (STRICT_SYNC and e != 'pe')):
                    E.wait_ge(self.sems[s2], self.sigcnt[s2])
                self.waited[e][s2] = self.issue[s2]
            for d, c in self.dcnt.items():
                if c > 0:
                    E.wait_ge(self.sems[d], 16 * c)
                self.waited[e][d] = c
        self.lastw = {}
        self.readers = {}
        self.lastop = {}


def build_nc(NPRE=NPRE, NTL=NTL):
    nc = bass.Bass("TRN2", target_bir_lowering=False)

    def din(name, shape):
        return nc.dram_tensor(name, list(shape), F32, kind="ExternalInput").ap()

    xl = din("xl", [NPRE + NTL, 128, 8 * NT])
    winl = din("winl", [7, 128, 8192])
    woutl = din("woutl", [128, 8192])
    wql = din("wql", [4, 128, 4096])
    subkl = din("subkl", [128, 2048])
    ul = din("ul", [NJG, 128, 8 * JG * 128])
    vl = din("vl", [NJG, 128, JG * 1024])
    wal = din("wal", [128, 1024])
    wxl = din("wxl", [128, 1024])
    chpl = din("chpl", [128, 8 * NPARAM])
    identl = din("identl", [128, 128])
    iotal = din("iotal", [128, 128])
    onesl = din("onesl", [128, 128])
    hflagl = din("hflagl", [128, 1])
    outl = nc.dram_tensor("outl", [NTL, 128, 8 * NT], F32, kind="ExternalOutput").ap()
    win_bf = nc.dram_tensor("win_bf", [7, 128, 8192], BF16, kind="Internal").ap()
    wq_bf = nc.dram_tensor("wq_bf", [4, 128, 4096], BF16, kind="Internal").ap()
    u_bf = nc.dram_tensor("u_bf", [NJG, 128, 8 * JG * 128], BF16, kind="Internal").ap()
    v_bf = nc.dram_tensor("v_bf", [NJG, 128, JG * 1024], BF16, kind="Internal").ap()

    with ExitStack() as es:
        S = Sch(nc, es)

        uid = [0]

        def sb(stack, name, shape, dt):
            uid[0] += 1
            return stack.enter_context(nc.sbuf_tensor(f"{name}_{uid[0]}", list(shape), dt))

        def cap(t, off, dims):
            full = t[:]
            ps = full.ap[0][0]
            return bass.AP(full.tensor, off, [[ps, 128]] + [list(d) for d in dims])

        PS = [es.enter_context(nc.psum_tensor(f"bank{i}", [128, 512], F32)) for i in range(8)]
        wout = sb(es, "wout", [128, 8192], BF16)
        subk = sb(es, "subk", [128, 2048], BF16)
        wa = sb(es, "wa", [128, 1024], BF16)
        wx = sb(es, "wx", [128, 1024], BF16)
        ident = sb(es, "ident", [128, 128], F32)
        iota = sb(es, "iota", [128, 128], F32)
        ones = sb(es, "ones", [128, 128], F32)
        chp = sb(es, "chp", [128, 8 * NPARAM], F32)
        hflag = sb(es, "hflag", [128, 1], F32)
        c8 = sb(es, "c8", [128, 8], F32)
        tmp8 = sb(es, "tmp8", [128, 8], F32)
        xa_halo = sb(es, "xa_halo", [128, 8 * 3], F32)
        cv_halo = sb(es, "cv_halo", [128, 8 * 2], F32)
        hst = sb(es, "hst", [128, 8], F32)
        qT = sb(es, "qT", [128, 16 * NT], BF16)
        hT = [sb(es, f"hT{i}", [128, 8 * NT], F32) for i in range(2)]
        xn2 = [sb(es, f"xn2_{i}", [128, 8 * NT], BF16) for i in range(2)]
        Wt = sb(es, "Wt", [128, NT * 128], BF16)
        iT = sb(es, "iT", [128, NT], F32)
        jT = sb(es, "jT", [128, NT], F32)
        gT = sb(es, "gT", [128, NT], F32)
        sq = [sb(es, f"sq{i}", [128, NT], F32) for i in range(2)]
        rt = sb(es, "rt", [128, NT], F32)
        rstd = sb(es, "rstd", [128, NT], F32)
        c8h = sb(es, "c8h", [128, 8], F32)
        iota_bf = sb(es, "iota_bf", [128, 128], BF16)
        hba = sb(es, "hba", [128, 8], F32)
        hbx = sb(es, "hbx", [128, 8], F32)

        woutv = wout[:].rearrange("p (k n) -> p k n", k=8)
        subkv = subk[:].rearrange("p (h n) -> p h n", h=16)
        wav = wa[:].rearrange("p (h n) -> p h n", h=8)
        wxv = wx[:].rearrange("p (h n) -> p h n", h=8)
        chpv = chp[:].rearrange("p (c k) -> p c k", c=8)
        xav = xa_halo[:].rearrange("p (c k) -> p c k", c=8)
        cvv = cv_halo[:].rearrange("p (c k) -> p c k", c=8)
        qTv = qT[:].rearrange("p (h t) -> p h t", h=16)
        hTvs = [t[:].rearrange("p (c t) -> p c t", c=8) for t in hT]
        xn2v = [t[:].rearrange("p (c t) -> p c t", c=8) for t in xn2]

        def par(c, k):
            return chpv[:, c, k:k + 1]

        bank_rr = [0]

        def nb():
            bank_rr[0] = bank_rr[0] % 7 + 1
            return bank_rr[0]

        def act_copy(out, in_, r, w):
            S.op('act', lambda E: E.activation(out=out, in_=in_, func=AF.Identity), r=r, w=w)

        def gelu_from(src, srckeys, dst, dstkey, tmps, tmpkeys, post=None):
            if not GELU_MANUAL:
                fn = AF.Gelu_apprx_tanh if GELU_TANH else AF.Gelu
                if post is None:
                    S.op('act', lambda E: E.activation(out=dst, in_=src, func=fn), r=srckeys, w=[dstkey])
                else:
                    pap, pkey = post
                    S.op('act', lambda E: E.activation(out=tmps[0], in_=src, func=fn), r=srckeys, w=[tmpkeys[0]])
                    S.op('dve', lambda E: E.tensor_tensor(out=dst, in0=tmps[0], in1=pap, op=ALU.mult),
                         r=[tmpkeys[0], pkey], w=[dstkey])
                return
            t0, t1 = tmps
            k0, k1 = tmpkeys
            S.op('act', lambda E: E.activation(out=t0, in_=src, func=AF.Square), r=srckeys, w=[k0])
            S.op('pool', lambda E: E.tensor_scalar(out=t0, in0=t0, scalar1=0.044715, scalar2=1.0, op0=ALU.mult,
                                                   op1=ALU.add), r=[k0], w=[k0])
            S.op('dve', lambda E: E.tensor_tensor(out=t0, in0=src, in1=t0, op=ALU.mult), r=srckeys + [k0], w=[k0])
            S.op('act', lambda E: E.activation(out=t0, in_=t0, func=AF.Sigmoid, scale=1.5957691216057308),
                 r=[k0], w=[k0])
            if post is None:
                S.op('dve', lambda E: E.tensor_tensor(out=dst, in0=src, in1=t0, op=ALU.mult), r=srckeys + [k0], w=[dstkey])
            else:
                pap, pkey = post
                S.op('dve', lambda E: E.tensor_tensor(out=t1, in0=src, in1=t0, op=ALU.mult), r=srckeys + [k0], w=[k1])
                S.op('pool', lambda E: E.tensor_tensor(out=dst, in0=t1, in1=pap, op=ALU.mult), r=[k1, pkey], w=[dstkey])

        for (dst, src, key) in [(chp, chpl, 'chp'), (ident, identl, 'ident'), (iota, iotal, 'iota'),
                                (ones, onesl, 'ones'), (hflag, hflagl, 'hflag')]:
            S.op('sp', lambda E: E.dma_start(out=dst[:], in_=src[:, :]), w=[key], dma='cst_' + key)
        S.op('sp', lambda E: E.dma_start(out=hT[0][:], in_=xl[0]), w=['hT0'], dma='dx0')
        S.op('pool', lambda E: E.memset(xa_halo[:], 0.0), w=['xa_halo'])
        S.op('pool', lambda E: E.memset(cv_halo[:], 0.0), w=['cv_halo'])
        S.op('pool', lambda E: E.memset(hst[:], 0.0), w=['hst'])
        S.op('act', lambda E: E.activation(out=tmp8[:], in_=chpv[:, :, K_LAM], func=AF.Exp, scale=-1.0),
             r=['chp'], w=['tmp8'])
        S.op('act', lambda E: E.activation(out=tmp8[:], in_=tmp8[:], func=AF.Ln, bias=1.0, scale=1.0),
             r=['tmp8'], w=['tmp8'])
        S.op('dve', lambda E: E.tensor_copy(out=iota_bf[:], in_=iota[:]), r=['iota'], w=['iota_bf'])
        S.op('dve', lambda E: E.tensor_scalar(out=c8h[:], in0=tmp8[:], scalar1=-4.0, scalar2=None, op0=ALU.mult),
             r=['tmp8'], w=['c8h'])
        S.op('dve', lambda E: E.tensor_scalar(out=hba[:], in0=chpv[:, :, K_BA], scalar1=0.5, scalar2=None, op0=ALU.mult),
             r=['chp'], w=['hba'])
        S.op('dve', lambda E: E.tensor_scalar(out=hbx[:], in0=chpv[:, :, K_BX], scalar1=0.5, scalar2=None, op0=ALU.mult),
             r=['chp'], w=['hbx'])

        with ExitStack() as pst, nc.named_scope("prepass"):
            NST = 3
            st32 = [sb(pst, f"st32_{i}", [128, 4096], F32) for i in range(NST)]
            st16 = [sb(pst, f"st16_{i}", [128, 4096], BF16) for i in range(NST)]
            pieces = []
            pieces.append((woutl[:, 0:4096], None, (wout[:, 0:4096], 'wout'), 4096))
            pieces.append((woutl[:, 4096:8192], None, (wout[:, 4096:8192], 'wout'), 4096))
            pieces.append((subkl[:, :], None, (subk[:], 'subk'), 2048))
            pieces.append((wal[:, :], None, (wa[:], 'wa'), 1024))
            pieces.append((wxl[:, :], None, (wx[:], 'wx'), 1024))
            for g in range(7):
                for hh_ in range(2):
                    pieces.append((winl[g][:, hh_ * 4096:(hh_ + 1) * 4096], win_bf[g][:, hh_ * 4096:(hh_ + 1) * 4096],
                                   None, 4096))
            for q in range(4):
                pieces.append((wql[q], wq_bf[q], None, 4096))
            for g in range(NJG):
                pieces.append((ul[g], u_bf[g], None, 4096))
                pieces.append((vl[g], v_bf[g], None, 4096))
            cengs = ['dve', 'act']

            def emit_load(i):
                src, ddst, sdst, n = pieces[i]
                b = i % NST
                S.op('sp', lambda E: E.dma_start(out=st32[b][:, :n], in_=src), w=[f'st32_{b}'], dma=f'pin{b}')

            for i in range(min(NST - 1, len(pieces))):
                emit_load(i)
            for i, (src, ddst, sdst, n) in enumerate(pieces):
                b = i % NST
                ce = cengs[i % 2]
                if sdst is not None:
                    o, okey = sdst
                else:
                    o, okey = st16[b][:, :n], f'st16_{b}'
                if ce == 'act':
                    S.op('act', lambda E: E.activation(out=o, in_=st32[b][:, :n], func=AF.Identity),
                         r=[f'st32_{b}'], w=[okey])
                else:
                    S.op(ce, lambda E: E.tensor_copy(out=o, in_=st32[b][:, :n]), r=[f'st32_{b}'], w=[okey])
                if i + NST - 1 < len(pieces):
                    emit_load(i + NST - 1)
                if ddst is not None:
                    S.op('sp', lambda E: E.dma_start(out=ddst, in_=st16[b][:, :n]), r=[okey], dma=f'pout{b}')
            S.barrier()

        def rmsnorm(srcv, skey, gk, dstv, dkey, slot=None, slotkey='b0'):
            if slot is None:
                slot = PS[0][:, :NT]
            for c in range(8):
                S.op('act', lambda E: E.activation(out=sq[c % 2][:], in_=srcv[:, c, :], func=AF.Square),
                     r=[skey], w=[f'sq{c % 2}'])
                S.op('pe', lambda E: E.matmul(slot, lhsT=ones[:], rhs=sq[c % 2][:],
                                              start=(c == 0), stop=(c == 7)),
                     r=['ones', f'sq{c % 2}'], w=[slotkey])
            S.op('act', lambda E: E.activation(out=rt[:], in_=slot, func=AF.Sqrt, bias=EPS, scale=1.0 / D),
                 r=[slotkey], w=['rt'])
            S.op('dve', lambda E: E.reciprocal(out=rstd[:], in_=rt[:]), r=['rt'], w=['rstd'])
            for c in range(8):
                S.op('dve', lambda E: E.scalar_tensor_tensor(out=dstv[:, c, :], in0=srcv[:, c, :], scalar=par(c, gk),
                                                             in1=rstd[:], op0=ALU.mult, op1=ALU.mult),
                     r=[skey, 'rstd', 'chp'], w=[dkey])

        def alloc_M(stk):
            M = {}
            M['win'] = [sb(stk, f"win{i}", [128, 8192], BF16) for i in range(2)]
            M['XA'] = sb(stk, "m_XA", [128, 8 * (NT + 4)], F32)
            M['CV'] = sb(stk, "m_CV", [128, 8 * (NT + 4)], F32)
            for nm in ['B2', 'B3', 'B4']:
                M[nm] = sb(stk, "m_" + nm, [128, 8 * NT], F32)
            M['xcb'] = sb(stk, "m_xcb", [128, 8 * NT], BF16)
            M['merged'] = M['xcb']
            M['wstate'] = {'g': [None, None], 'n': 0}
            return M

        def phase_M(ti, full, lastpre, M):
            hTv = hTvs[ti % 2]
            khT = f'hT{ti % 2}'
            xb = hTv
            kx = khT
            xnv = xn2v[ti % 2]
            kxn = f'xn2_{ti % 2}'

            def next_x():
                if ti + 1 < NPRE + NTL and ti <= NPRE:
                    nb_ = (ti + 1) % 2
                    S.op('sp', lambda E: E.dma_start(out=hT[nb_][:], in_=xl[ti + 1]), w=[f'hT{nb_}'], dma=f'dx{nb_}')
            needall = full or lastpre
            winv = [w_[:].rearrange("p (d n) -> p d n", d=8) for w_ in M['win']]
            ws = M['wstate']
            XA = M['XA'][:].rearrange("p (c t) -> p c t", c=8)
            CV = M['CV'][:].rearrange("p (c t) -> p c t", c=8)
            XC = M['B2'][:].rearrange("p (c t) -> p c t", c=8)
            RA = M['B3'][:].rearrange("p (c t) -> p c t", c=8)
            IG = M['B4'][:].rearrange("p (c t) -> p c t", c=8)
            T1 = XA[:, :, 0:NT]
            xcbv = M['xcb'][:].rearrange("p (c t) -> p c t", c=8)
            mergedv = M['merged'][:].rearrange("p (c t) -> p c t", c=8)
            glist = [0] + ([2, 4] if needall else []) + ([3, 1, 5, 6] if full else [])

            def ensure_w(g):
                for b in range(2):
                    if ws['g'][b] == g:
                        return b
                b = ws['n'] % 2
                ws['n'] += 1
                ws['g'][b] = g
                S.op('sp', lambda E: E.dma_start(out=M['win'][b][:], in_=win_bf[g]), w=[f'win{b}'], dma=f'dw{b}')
                return b

            def proj(g, c):
                b = ensure_w(g)
                bk = nb()
                for dc in range(8):
                    S.op('pe', lambda E: E.matmul(PS[bk][:, :NT], lhsT=winv[b][:, dc, c * 128:(c + 1) * 128],
                                                  rhs=xnv[:, dc, :], start=(dc == 0), stop=(dc == 7)),
                         r=[f'win{b}', kxn], w=[f'b{bk}'])
                return bk

            def stage_begin(g):
                ensure_w(g)
                i_ = glist.index(g)
                if i_ + 1 < len(glist):
                    ensure_w(glist[i_ + 1])

            ensure_w(glist[0])
            rmsnorm(xb, kx, K_G1, xnv, kxn)
            stage_begin(0)
            S.op('pool', lambda E: E.tensor_copy(out=XA[:, :, 0:3], in_=xav), r=['xa_halo'], w=['XA'])
            for c in range(8):
                bk = proj(0, c)
                act_copy(XA[:, c, 3:3 + NT], PS[bk][:, :NT], [f'b{bk}'], ['XA'])
            S.op('pool', lambda E: E.tensor_copy(out=xav, in_=XA[:, :, NT:NT + 3]), r=['XA'], w=['xa_halo'])
            for c in range(8):
                S.op('dve', lambda E: E.tensor_scalar(out=XC[:, c, :], in0=XA[:, c, 0:NT], scalar1=par(c, K_CAW),
                                                      scalar2=par(c, K_CAB), op0=ALU.mult, op1=ALU.add),
                     r=['XA', 'chp'], w=['B2'])
            for k in range(1, 4):
                for c in range(8):
                    S.op('dve', lambda E: E.scalar_tensor_tensor(out=XC[:, c, :], in0=XA[:, c, k:k + NT],
                                                                 scalar=par(c, K_CAW + k), in1=XC[:, c, :],
                                                                 op0=ALU.mult, op1=ALU.add),
                         r=['XA', 'chp', 'B2'], w=['B2'])
            act_copy(M['xcb'][:], M['B2'][:], ['B2'], ['xcb'])
            for c in range(8):
                b1 = nb()
                S.op('pe', lambda E: E.matmul(PS[b1][:, :NT], lhsT=wav[:, c, :], rhs=xcbv[:, c, :], start=True, stop=True),
                     r=['wa', 'xcb'], w=[f'b{b1}'])
                b2 = nb()
                S.op('pe', lambda E: E.matmul(PS[b2][:, :NT], lhsT=wxv[:, c, :], rhs=xcbv[:, c, :], start=True, stop=True),
                     r=['wx', 'xcb'], w=[f'b{b2}'])
                S.op('act', lambda E: E.activation(out=RA[:, c, :], in_=PS[b1][:, :NT], func=AF.Tanh,
                                                   bias=hba[:, c:c + 1], scale=0.5),
                     r=[f'b{b1}', 'hba'], w=['B3'])
                S.op('act', lambda E: E.activation(out=IG[:, c, :], in_=PS[b2][:, :NT], func=AF.Tanh,
                                                   bias=hbx[:, c:c + 1], scale=0.5),
                     r=[f'b{b2}', 'hbx'], w=['B4'])
            for c in range(8):
                S.op('act', lambda E: E.activation(out=RA[:, c, :], in_=RA[:, c, :], func=AF.Exp,
                                                   bias=c8h[:, c:c + 1], scale=c8h[:, c:c + 1]),
                     r=['B3', 'c8h'], w=['B3'])
            S.op('act', lambda E: E.activation(out=T1, in_=RA, func=AF.Square), r=['B3'], w=['XA'])
            S.op('dve', lambda E: E.tensor_scalar(out=T1, in0=T1, scalar1=1.0, scalar2=-1.0,
                                                  op0=ALU.min, op1=ALU.mult), r=['XA'], w=['XA'])
            S.op('dve', lambda E: E.tensor_scalar(out=M['B4'][:], in0=M['B4'][:], scalar1=0.5, scalar2=0.5,
                                                  op0=ALU.mult, op1=ALU.add), r=['B4'], w=['B4'])
            S.op('act', lambda E: E.activation(out=T1, in_=T1, func=AF.Sqrt, bias=1.0, scale=1.0),
                 r=['XA'], w=['XA'])
            S.op('dve', lambda E: E.tensor_tensor(out=M['B2'][:], in0=M['B2'][:], in1=M['B4'][:], op=ALU.mult),
                 r=['B2', 'B4'], w=['B2'])
            S.op('dve', lambda E: E.tensor_tensor(out=XC, in0=XC, in1=T1, op=ALU.mult),
                 r=['B2', 'XA'], w=['B2'])
            for c in range(8):
                S.op('dve', lambda E: E.tensor_tensor_scan(out=T1[:, c, :], data0=RA[:, c, :], data1=XC[:, c, :],
                                                           initial=hst[:, c:c + 1], op0=ALU.mult, op1=ALU.add),
                     r=['B3', 'B2', 'hst'], w=['XA'])
            if lastpre:
                S.op('pool', lambda E: E.tensor_scalar(out=hst[:], in0=T1[:, :, NT - 1], scalar1=hflag[:, 0:1],
                                                       scalar2=None, op0=ALU.mult),
                     r=['XA', 'hflag'], w=['hst'])
            else:
                S.op('pool', lambda E: E.tensor_copy(out=hst[:], in_=T1[:, :, NT - 1]), r=['XA'], w=['hst'])
            if not needall:
                next_x()
                return
            VB = XC
            stage_begin(2)
            for c in range(8):
                bk = proj(2, c)
                act_copy(VB[:, c, :], PS[bk][:, :NT], [f'b{bk}'], ['B2'])
            S.op('pool', lambda E: E.tensor_copy(out=CV[:, :, 0:2], in_=cvv), r=['cv_halo'], w=['CV'])
            stage_begin(4)
            for c in range(8):
                bk = proj(4, c)
                S.op('dve', lambda E: E.tensor_tensor(out=CV[:, c, 2:2 + NT], in0=PS[bk][:, :NT], in1=VB[:, c, :],
                                                      op=ALU.mult),
                     r=[f'b{bk}', 'B2'], w=['CV'])
            S.op('pool', lambda E: E.tensor_copy(out=cvv, in_=CV[:, :, NT:NT + 2]), r=['CV'], w=['cv_halo'])
            if not full:
                next_x()
                return
            YCV = RA
            for c in range(8):
                S.op('dve', lambda E: E.tensor_scalar(out=YCV[:, c, :], in0=CV[:, c, 0:NT], scalar1=par(c, K_CBW),
                                                      scalar2=None, op0=ALU.mult),
                     r=['CV', 'chp'], w=['B3'])
            for k in range(1, 3):
                for c in range(8):
                    S.op('dve', lambda E: E.scalar_tensor_tensor(out=YCV[:, c, :], in0=CV[:, c, k:k + NT],
                                                                 scalar=par(c, K_CBW + k), in1=YCV[:, c, :],
                                                                 op0=ALU.mult, op1=ALU.add),
                         r=['CV', 'chp', 'B3'], w=['B3'])
            stage_begin(3)
            for c in range(8):
                bk = proj(3, c)
                S.op('dve', lambda E: E.tensor_tensor(out=YCV[:, c, :], in0=PS[bk][:, :NT], in1=YCV[:, c, :], op=ALU.mult),
                     r=[f'b{bk}', 'B3'], w=['B3'])
            GG = IG
            stage_begin(1)
            for c in range(8):
                bk = proj(1, c)
                S.op('act', lambda E: E.activation(out=GG[:, c, :], in_=PS[bk][:, :NT], func=AF.Gelu_apprx_tanh),
                     r=[f'b{bk}'], w=['B4'])
            SA = CV
            SB = XC
            stage_begin(5)
            for c in range(8):
                bk = proj(5, c)
                S.op('act', lambda E: E.activation(out=SA[:, c, 0:NT], in_=PS[bk][:, :NT], func=AF.Tanh, scale=0.5),
                     r=[f'b{bk}'], w=['CV'])
            stage_begin(6)
            for c in range(8):
                bk = proj(6, c)
                S.op('act', lambda E: E.activation(out=SB[:, c, :], in_=PS[bk][:, :NT], func=AF.Tanh, scale=0.5),
                     r=[f'b{bk}'], w=['B2'])
            S.op('dve', lambda E: E.tensor_tensor(out=GG, in0=GG, in1=T1, op=ALU.mult), r=['B4', 'XA'], w=['B4'])
            S.op('dve', lambda E: E.tensor_scalar(out=SA[:, :, 0:NT], in0=SA[:, :, 0:NT], scalar1=0.5, scalar2=0.5,
                                                  op0=ALU.mult, op1=ALU.add), r=['CV'], w=['CV'])
            S.op('dve', lambda E: E.tensor_scalar(out=SB, in0=SB, scalar1=0.5, scalar2=0.5,
                                                  op0=ALU.mult, op1=ALU.add), r=['B2'], w=['B2'])
            S.op('dve', lambda E: E.tensor_tensor(out=GG, in0=GG, in1=SA[:, :, 0:NT], op=ALU.mult),
                 r=['B4', 'CV'], w=['B4'])
            S.op('dve', lambda E: E.tensor_tensor(out=YCV, in0=YCV, in1=SB, op=ALU.mult), r=['B3', 'B2'], w=['B3'])
            S.op('dve', lambda E: E.tensor_tensor(out=mergedv, in0=GG, in1=YCV, op=ALU.add),
                 r=['B4', 'B3'], w=['xcb'])
            for oc in range(8):
                bk = nb()
                for kc in range(8):
                    S.op('pe', lambda E: E.matmul(PS[bk][:, :NT], lhsT=woutv[:, kc, oc * 128:(oc + 1) * 128],
                                                  rhs=mergedv[:, kc, :], start=(kc == 0), stop=(kc == 7)),
                         r=['wout', 'xcb'], w=[f'b{bk}'])
                S.op('dve', lambda E: E.tensor_tensor(out=hTv[:, oc, :], in0=PS[bk][:, :NT], in1=hTv[:, oc, :], op=ALU.add),
                     r=[f'b{bk}', khT], w=[khT])
            next_x()
            for piece in range(4):
                b = piece // 2
                lo = (piece % 2) * 4096
                S.op('sp', lambda E: E.dma_start(out=M['win'][b][:, lo:lo + 4096], in_=wq_bf[piece]),
                     w=[f'win{b}'], dma=f'dw{b}')
            rmsnorm(hTv, khT, K_G2, xnv, kxn)
            for hp in range(16):
                piece, k = hp // 4, hp % 4
                b = piece // 2
                wq_ = M['win'][b][:, (piece % 2) * 4096 + k * 1024:(piece % 2) * 4096 + (k + 1) * 1024]
                wqv_ = wq_.rearrange("p (d n) -> p d n", d=8)
                bk = nb()
                for dc in range(8):
                    S.op('pe', lambda E: E.matmul(PS[bk][:, :NT], lhsT=wqv_[:, dc, :], rhs=xnv[:, dc, :],
                                                  start=(dc == 0), stop=(dc == 7)),
                         r=[f'win{b}', kxn], w=[f'b{bk}'])
                act_copy(qTv[:, hp, :], PS[bk][:, :NT], [f'b{bk}'], ['qT'])

        def alloc_P1a(stk):
            T = {}
            T['sc'] = sb(stk, "sc", [128, 2048], F32)
            T['m'] = sb(stk, "m", [128, 256], F32)
            T['iu'] = sb(stk, "iu", [128, 256], U32)
            T['idf'] = sb(stk, "idf", [128, 256], F32)
            T['wk1s'] = [sb(stk, f"wk1_{i}", [128, 128], F32) for i in range(2)]
            T['cand'] = sb(stk, "cand", [128, 2048], F32)
            T['wk2s'] = [sb(stk, f"wk2_{i}", [128, 256], F32) for i in range(2)]
            for nm, dt_ in [('c16', F32), ('pu', U32), ('au', U32), ('bu', U32), ('af', F32), ('bf_', F32),
                            ('e16', F32), ('gsel', F32), ('isel', F32), ('jsel', F32)]:
                T[nm] = sb(stk, nm, [128, 128], dt_)
            for nm in ['negmax', 'Z', 'rz']:
                T[nm] = sb(stk, nm, [128, 8], F32)
            return T

        def gen_P1a(ti, T, slots):
            par_ = ti % 2
            hTv = hTvs[par_]
            khT = f'hT{par_}'
            x2v = xn2v[par_]
            kx2 = f'xn2_{par_}'
            (nslot, nkey), xs = slots
            xi = [0]

            def nslot_():
                xi[0] = (xi[0] + 1) % len(xs)
                return xs[xi[0]]

            sc, m, iu, idf = T['sc'], T['m'], T['iu'], T['idf']
            wk1s, cand, wk2s = T['wk1s'], T['cand'], T['wk2s']
            c16, pu, au, bu, af, bf_ = T['c16'], T['pu'], T['au'], T['bu'], T['af'], T['bf_']
            negmax, Z, rz, e16, gsel, isel, jsel = T['negmax'], T['Z'], T['rz'], T['e16'], T['gsel'], T['isel'], T['jsel']
            scv = sc[:].rearrange("p (h n) -> p h n", h=16)
            mv = m[:].rearrange("p (h k) -> p h k", h=16)
            iuv = iu[:].rearrange("p (h k) -> p h k", h=16)
            candv = cand[:].rearrange("p (h a b) -> p h a b", h=8, a=16)
            cand3 = cand[:].rearrange("p (h ab) -> p h ab", h=8)
            c16v = c16[:].rearrange("p (h k) -> p h k", h=8)
            puv = pu[:].rearrange("p (h k) -> p h k", h=8)
            e16v = e16[:].rearrange("p (h k) -> p h k", h=8)
            gselv = gsel[:].rearrange("p (h k) -> p h k", h=8)
            PU = [f'pu{h}' for h in range(8)]
            C16 = [f'c16_{h}' for h in range(8)]

            for s in range(NT // 128):
                for hp4 in range(4):
                    for k2 in range(2):
                        sl, skeys = nslot_()
                        for k in range(2):
                            hp = hp4 * 4 + k2 * 2 + k
                            S.op('pe', lambda E: E.matmul(sl(k * 128, 128), lhsT=qTv[:, hp, s * 128:(s + 1) * 128],
                                                          rhs=subkv[:, hp, :], start=True, stop=True),
                                 r=['qT', 'subk'], w=skeys)
                        c0 = (hp4 * 4 + k2 * 2) * 128
                        act_copy(sc[:, c0:c0 + 256], sl(0, 256), skeys, ['sc'])
                    yield
                for hp0 in range(0, 16, 2):
                    pr = [(hp0, wk1s[0], 'wk1_0'), (hp0 + 1, wk1s[1], 'wk1_1')]
                    for hp, wk1, kk in pr:
                        S.op('dve', lambda E: E.max(out=mv[:, hp, 0:8], in_=scv[:, hp, :]), r=['sc'], w=[f'm{hp}'])
                    for hp, wk1, kk in pr:
                        S.op('dve', lambda E: E.max_index(out=iuv[:, hp, 0:8], in_max=mv[:, hp, 0:8],
                                                          in_values=scv[:, hp, :]), r=['sc', f'm{hp}'], w=[f'iu{hp}'])
                    for hp, wk1, kk in pr:
                        S.op('dve', lambda E: E.match_replace(out=wk1[:], in_to_replace=mv[:, hp, 0:8],
                                                              in_values=scv[:, hp, :], imm_value=-1e30),
                             r=['sc', f'm{hp}'], w=[kk])
                    for hp, wk1, kk in pr:
                        S.op('dve', lambda E: E.max(out=mv[:, hp, 8:16], in_=wk1[:]), r=[kk], w=[f'm{hp}'])
                    for hp, wk1, kk in pr:
                        S.op('dve', lambda E: E.max_index(out=iuv[:, hp, 8:16], in_max=mv[:, hp, 8:16], in_values=wk1[:]),
                             r=[kk, f'm{hp}'], w=[f'iu{hp}'])
                    yield
                S.op('dve', lambda E: E.tensor_copy(out=idf[:], in_=iu[:]), r=[f'iu{q}' for q in range(16)], w=['idf'])
                S.op('dve', lambda E: E.tensor_tensor(out=candv, in0=cap(m, 0, [[32, 8], [1, 16], [0, 16]]),
                                                      in1=cap(m, 16, [[32, 8], [0, 16], [1, 16]]), op=ALU.add),
                     r=[f'm{q}' for q in range(16)], w=['cand'])
                yield
                for h0 in range(0, 8, 2):
                    pr = [(h0, wk2s[0], 'wk2_0'), (h0 + 1, wk2s[1], 'wk2_1')]
                    for h, wk2, kk in pr:
                        S.op('dve', lambda E: E.max(out=c16v[:, h, 0:8], in_=cand3[:, h, :]), r=['cand'], w=[f'c16_{h}'])
                    for h, wk2, kk in pr:
                        S.op('dve', lambda E: E.max_index(out=puv[:, h, 0:8], in_max=c16v[:, h, 0:8],
                                                          in_values=cand3[:, h, :]), r=['cand', f'c16_{h}'], w=[f'pu{h}'])
                    for h, wk2, kk in pr:
                        S.op('dve', lambda E: E.match_replace(out=wk2[:], in_to_replace=c16v[:, h, 0:8],
                                                              in_values=cand3[:, h, :], imm_value=-1e30),
                             r=['cand', f'c16_{h}'], w=[kk])
                    for h, wk2, kk in pr:
                        S.op('dve', lambda E: E.max(out=c16v[:, h, 8:16], in_=wk2[:]), r=[kk], w=[f'c16_{h}'])
                    for h, wk2, kk in pr:
                        S.op('dve', lambda E: E.max_index(out=puv[:, h, 8:16], in_max=c16v[:, h, 8:16], in_values=wk2[:]),
                             r=[kk, f'c16_{h}'], w=[f'pu{h}'])
                    yield
                S.op('dve', lambda E: E.tensor_single_scalar(out=au[:], in_=pu[:], scalar=4, op=ALU.logical_shift_right),
                     r=PU, w=['au'])
                S.op('dve', lambda E: E.tensor_single_scalar(out=bu[:], in_=pu[:], scalar=15, op=ALU.bitwise_and),
                     r=PU, w=['bu'])
                S.op('dve', lambda E: E.tensor_copy(out=af[:], in_=au[:]), r=['au'], w=['af'])
                S.op('dve', lambda E: E.tensor_copy(out=bf_[:], in_=bu[:]), r=['bu'], w=['bf_'])
                S.op('dve', lambda E: E.tensor_scalar(out=negmax[:], in0=c16v[:, :, 0], scalar1=-1.0, scalar2=None,
                                                      op0=ALU.mult), r=C16, w=['negmax'])
                for h in range(8):
                    S.op('act', lambda E: E.activation(out=e16v[:, h, :], in_=c16v[:, h, :], func=AF.Exp,
                                                       bias=negmax[:, h:h + 1], scale=1.0),
                         r=[f'c16_{h}', 'negmax'], w=['e16'])
                yield
                S.op('dve', lambda E: E.tensor_reduce(out=Z[:], in_=e16v, axis=AX.X, op=ALU.add), r=['e16'], w=['Z'])
                S.op('dve', lambda E: E.reciprocal(out=rz[:], in_=Z[:]), r=['Z'], w=['rz'])
                S.op('dve', lambda E: E.tensor_tensor(out=gselv, in0=e16v, in1=cap(rz, 0, [[1, 8], [0, 16]]), op=ALU.mult),
                     r=['e16', 'rz'], w=['gsel'])
                oh3 = cand[:].rearrange("p (k a) -> p k a", a=16)
                oh4 = cand[:].rearrange("p (h k a) -> p h k a", h=8, k=16)
                for (sel, selkey, abf, abkey, off) in [(isel, 'isel', af, 'af', 0), (jsel, 'jsel', bf_, 'bf_', 16)]:
                    S.op('dve', lambda E: E.tensor_tensor(out=oh3, in0=cap(iota, 0, [[0, 128], [1, 16]]),
                                                          in1=cap(abf, 0, [[1, 128], [0, 16]]), op=ALU.is_equal),
                         r=['iota', abkey], w=['cand'])
                    yield
                    S.op('dve', lambda E: E.tensor_tensor(out=oh4, in0=oh4, in1=cap(idf, off, [[32, 8], [0, 16], [1, 16]]),
                                                          op=ALU.mult),
                         r=['cand', 'idf'], w=['cand'])
                    S.op('dve', lambda E: E.tensor_reduce(out=sel[:], in_=oh3, axis=AX.X, op=ALU.add),
                         r=['cand'], w=[selkey])
                    yield
                for (src, skey, dstT, dkey) in [(isel, 'isel', iT, 'iT'), (jsel, 'jsel', jT, 'jT'), (gsel, 'gsel', gT, 'gT')]:
                    sl, skeys = nslot_()
                    S.op('pe', lambda E: E.transpose(out=sl(0, 128), in_=src[:], identity=ident[:]),
                         r=[skey, 'ident'], w=skeys)
                    act_copy(dstT[:, s * 128:(s + 1) * 128], sl(0, 128), skeys, [dkey])
                yield

        def mkslot(bank, half=0):
            return (lambda off, n: PS[bank][:, off: off + n]), [f'b{bank}']

        def phase_WB(ti, stk):
            OH1 = [sb(stk, f"OH1_{i}", [128, TB * 128], BF16) for i in range(2)]
            OH2 = [sb(stk, f"OH2_{i}", [128, TB * 128], BF16) for i in range(2)]
            wbanks = [1, 2, 3, 4]
            for tb in range(NT // TB):
                t0 = tb * TB
                ob = tb % 2
                o1 = OH1[ob][:].rearrange("p (t i) -> p t i", t=TB)
                o2 = OH2[ob][:].rearrange("p (t i) -> p t i", t=TB)
                k1, k2 = f'OH1_{ob}', f'OH2_{ob}'
                for tt in range(TB):
                    t = t0 + tt
                    S.op('dve', lambda E: E.tensor_scalar(out=o1[:, tt, :], in0=iota_bf[:], scalar1=iT[:, t:t + 1],
                                                          scalar2=gT[:, t:t + 1], op0=ALU.is_equal, op1=ALU.mult),
                         r=['iota_bf', 'iT', 'gT'], w=[k1])
                    S.op('dve', lambda E: E.tensor_scalar(out=o2[:, tt, :], in0=iota_bf[:], scalar1=jT[:, t:t + 1],
                                                          scalar2=None, op0=ALU.is_equal),
                         r=['iota_bf', 'jT'], w=[k2])
                for tt in range(TB):
                    t = t0 + tt
                    bk = wbanks[(t // 4) % 4]
                    S.op('pe', lambda E: E.matmul(PS[bk][:, (t % 4) * 128:(t % 4 + 1) * 128], lhsT=o1[:, tt, :],
                                                  rhs=o2[:, tt, :], start=True, stop=True),
                         r=[k1, k2], w=[f'b{bk}'])
                    if t % 4 == 3:
                        act_copy(Wt[:, (t - 3) * 128:(t + 1) * 128], PS[bk][:, :512], [f'b{bk}'], ['Wt'])

        def phase_P2(ti, stk, gen):
            par_ = ti % 2
            hTv = hTvs[par_]
            khT = f'hT{par_}'
            x2v = xn2v[par_]
            kx2 = f'xn2_{par_}'
            U = [sb(stk, f"U{i}", [128, 8 * JG * 128], BF16) for i in range(2)]
            Vv = [sb(stk, f"V{i}", [128, JG * 1024], BF16) for i in range(2)]
            gt0 = [sb(stk, f"gt0_{i}", [128, NT], F32) for i in range(2)]
            Cb = [sb(stk, f"C{i}", [128, JG * NT], BF16) for i in range(2)]
            Uv = [u_[:].rearrange("p (d j i) -> p d j i", d=8, j=JG) for u_ in U]
            Vvv = [v_[:].rearrange("p (j d) -> p j d", j=JG) for v_ in Vv]
            Cv = [c_[:].rearrange("p (j t) -> p j t", j=JG) for c_ in Cb]
            aslots = [mkslot(0), mkslot(1)]

            def load(g):
                b = g % 2
                S.op('sp', lambda E: E.dma_start(out=U[b][:], in_=u_bf[g]), w=[f'U{b}'], dma=f'du{b}')
                S.op('sp', lambda E: E.dma_start(out=Vv[b][:], in_=v_bf[g]), w=[f'V{b}'], dma=f'dv{b}')

            def pull(n):
                if gen is None:
                    return
                for _ in range(n):
                    try:
                        next(gen)
                    except StopIteration:
                        return

            if NO_INTERLEAVE:
                pull(10000)
            else:
                pull(PRE_PULL)
            def loadU(g):
                b = g % 2
                S.op('sp', lambda E: E.dma_start(out=U[b][:], in_=u_bf[g]), w=[f'U{b}'], dma=f'du{b}')

            def loadV(g):
                b = g % 2
                S.op('sp', lambda E: E.dma_start(out=Vv[b][:], in_=v_bf[g]), w=[f'V{b}'], dma=f'dv{b}')

            def vside(g, dcs):
                b = g % 2
                for dc in dcs:
                    bk = 4 + dc // 2
                    lo = (dc % 2) * NT
                    for jj in range(JG):
                        S.op('pe', lambda E: E.matmul(PS[bk][:, lo:lo + NT], lhsT=Vvv[b][:, jj, dc * 128:(dc + 1) * 128],
                                                      rhs=Cv[b][:, jj, :],
                                                      start=(g == 0 and jj == 0 and dc % 2 == 0),
                                                      stop=(g == NJG - 1 and jj == JG - 1),
                                                      skip_group_check=True),
                             r=[f'V{b}', f'C{b}'], w=[f'pb{4 + dc // 2}'])

            loadU(0)
            loadV(0)
            loadV(1)
            na = 0
            for jg in range(NJG):
                if jg + 1 < NJG:
                    loadU(jg + 1)
                b = jg % 2
                for jj in range(JG):
                    j = jg * JG + jj
                    sl, skeys = aslots[na % 2]
                    na += 1
                    for dc in range(8):
                        S.op('pe', lambda E: E.matmul(sl(0, NT), lhsT=Uv[b][:, dc, jj, :], rhs=x2v[:, dc, :],
                                                      start=(dc == 0), stop=(dc == 7)),
                             r=[f'U{b}', kx2], w=skeys)
                    gb = jj % 2
                    S.op('act', lambda E: E.activation(out=gt0[gb][:], in_=sl(0, NT), func=AF.Gelu_apprx_tanh),
                         r=skeys, w=[f'gt0_{gb}'])
                    S.op('dve', lambda E: E.tensor_tensor(out=Cv[b][:, jj, :], in0=gt0[gb][:],
                                                          in1=cap(Wt, j, [[128, NT]]), op=ALU.mult),
                         r=[f'gt0_{gb}', 'Wt'], w=[f'C{b}'])
                    if jg > 0:
                        vside(jg - 1, [2 * jj, 2 * jj + 1])
                if jg > 0 and jg + 1 < NJG:
                    loadV(jg + 1)
                pull(2)
            vside(NJG - 1, list(range(8)))
            pull(10000)
            for dc in range(8):
                bk = 4 + dc // 2
                lo = (dc % 2) * NT
                S.op('dve', lambda E: E.tensor_tensor(out=hTv[:, dc, :], in0=PS[bk][:, lo:lo + NT], in1=hTv[:, dc, :],
                                                      op=ALU.add),
                     r=[f'pb{4 + dc // 2}', khT], w=[khT])
            rmsnorm(hTv, khT, K_GF, hTv, khT, slot=PS[2][:, :NT], slotkey='b2')
            S.op('sp', lambda E: E.dma_start(out=outl[ti - NPRE], in_=hT[par_][:]), r=[khT], dma='dout')
            if ti + 2 < NPRE + NTL:
                S.op('sp', lambda E: E.dma_start(out=hT[par_][:], in_=xl[ti + 2]), w=[khT], dma=f'dx{par_}')

        p1slots = ((PS[2][:, :NT], 'b2'), [mkslot(3)])
        with ExitStack() as stk:
            M = alloc_M(stk)
            for ti in range(NPRE):
                with nc.named_scope(f"pre{ti}"):
                    phase_M(ti, False, ti == NPRE - 1, M)
            S.barrier()
        t_first, t_last = NPRE, NPRE + NTL - 1
        with ExitStack() as stk:
            M = alloc_M(stk)
            with nc.named_scope(f"M{t_first}"):
                phase_M(t_first, True, False, M)
            S.barrier()
        with ExitStack() as stk:
            T = alloc_P1a(stk)
            with nc.named_scope(f"P1a_{t_first}"):
                for _ in gen_P1a(t_first, T, p1slots):
                    pass
            S.barrier()
        with ExitStack() as stk:
            with nc.named_scope(f"WB_{t_first}"):
                phase_WB(t_first, stk)
            S.barrier()
        for ti in range(t_first, t_last + 1):
            more = ti + 1 <= t_last
            if more:
                with ExitStack() as stk:
                    M = alloc_M(stk)
                    with nc.named_scope(f"M{ti + 1}"):
                        phase_M(ti + 1, True, False, M)
                    S.barrier()
            with ExitStack() as stk:
                gen = None
                if more:
                    T = alloc_P1a(stk)
                    gen = gen_P1a(ti + 1, T, p1slots)
                with nc.named_scope(f"OV_{ti}"):
                    phase_P2(ti, stk, gen)
                S.barrier()
            if more:
                with ExitStack() as stk:
                    with nc.named_scope(f"WB_{ti + 1}"):
                        phase_WB(ti + 1, stk)
                    S.barrier()
        nc.sync.wait_ge(S.sems['dout'], 16 * S.dcnt['dout'])
    return nc


_NC_CACHE = {}


def _layouts(x, norm1_g, w_in, conv_a_w, conv_a_b, w_a, b_a, w_x, b_x, lru_lambda, conv_b_w, w_out,
             norm2_g, peer_wq, peer_subkeys, peer_u, peer_v, final_g):
    f = np.float32
    shared = {}
    w = np.asarray(w_in[0], f).reshape(8, 128, 7, 1024)
    shared["winl"] = np.ascontiguousarray(w.transpose(2, 1, 0, 3)).reshape(7, 128, 8192)
    shared["woutl"] = np.ascontiguousarray(np.asarray(w_out[0], f).reshape(8, 128, 1024).transpose(1, 0, 2)).reshape(128, 8192)
    wq = np.asarray(peer_wq[0], f).reshape(8, 128, 4, 4, 128)
    shared["wql"] = np.ascontiguousarray(wq.transpose(2, 1, 3, 0, 4)).reshape(4, 128, 4096)
    sk = np.asarray(peer_subkeys[0], f).reshape(16, 128, 128)
    shared["subkl"] = np.ascontiguousarray(sk.transpose(2, 0, 1)).reshape(128, 2048)
    u = np.asarray(peer_u[0], f).reshape(128, NJG, JG, 8, 128)
    shared["ul"] = np.ascontiguousarray(u.transpose(1, 4, 3, 2, 0)).reshape(NJG, 128, 8 * JG * 128)
    v = np.asarray(peer_v[0], f).reshape(128, NJG, JG, 1024)
    shared["vl"] = np.ascontiguousarray(v.transpose(1, 0, 2, 3)).reshape(NJG, 128, JG * 1024)
    shared["wal"] = np.ascontiguousarray(np.asarray(w_a[0], f).transpose(1, 0, 2)).reshape(128, 1024)
    shared["wxl"] = np.ascontiguousarray(np.asarray(w_x[0], f).transpose(1, 0, 2)).reshape(128, 1024)
    chp = np.zeros((128, 8, NPARAM), f)

    def pc(vec):
        return np.asarray(vec, f).reshape(8, 128).T

    chp[:, :, K_G1] = pc(norm1_g[0])
    for k in range(4):
        chp[:, :, K_CAW + k] = pc(conv_a_w[0][k])
    chp[:, :, K_CAB] = pc(conv_a_b[0])
    chp[:, :, K_BA] = pc(b_a[0])
    chp[:, :, K_BX] = pc(b_x[0])
    chp[:, :, K_LAM] = pc(lru_lambda[0])
    for k in range(3):
        chp[:, :, K_CBW + k] = pc(conv_b_w[0][k])
    chp[:, :, K_G2] = pc(norm2_g[0])
    chp[:, :, K_GF] = pc(final_g)
    shared["chpl"] = chp.reshape(128, 8 * NPARAM)
    shared["identl"] = np.eye(128, dtype=f)
    shared["iotal"] = np.ascontiguousarray(np.broadcast_to(np.arange(128, dtype=f)[None, :], (128, 128)))
    shared["onesl"] = np.ones((128, 128), f)
    return shared


def kernel(x, norm1_g, w_in, conv_a_w, conv_a_b, w_a, b_a, w_x, b_x, lru_lambda, conv_b_w, w_out,
           norm2_g, peer_wq, peer_subkeys, peer_u, peer_v, final_g):
    f = np.float32
    x = np.asarray(x, f)
    B, Sq, _ = x.shape
    shared = _layouts(x, norm1_g, w_in, conv_a_w, conv_a_b, w_a, b_a, w_x, b_x, lru_lambda, conv_b_w, w_out,
                      norm2_g, peer_wq, peer_subkeys, peer_u, peer_v, final_g)
    in_maps = []
    for core in range(8):
        b, half = core // 2, core % 2
        win = np.zeros((2 * TOK, D), f)
        win[TOK:] = x[b, half * TOK:(half + 1) * TOK]
        if half == 1:
            win[:TOK] = x[b, 0:TOK]
        xl = np.ascontiguousarray(win.reshape(NPRE + NTL, NT, 8, 128).transpose(0, 3, 2, 1)).reshape(NPRE + NTL, 128, 8 * NT)
        m = dict(shared)
        m["xl"] = xl
        m["hflagl"] = np.full((128, 1), float(half), f)
        in_maps.append(m)
    if "nc" not in _NC_CACHE:
        _NC_CACHE["nc"] = build_nc()
    nc = _NC_CACHE["nc"]
    res = run_bass_kernel_spmd(nc, in_maps, core_ids=list(range(8)))
    out = np.zeros((B, Sq, D), f)
    for core in range(8):
        b, half = core // 2, core % 2
        o = np.asarray(res.results[core]["outl"], f).reshape(NTL, 128, 8, NT)
        o = o.transpose(0, 3, 2, 1).reshape(TOK, D)
        out[b, half * TOK:(half + 1) * TOK] = o
    return out
```

```python
import numpy as np
from contextlib import ExitStack
import concourse.bass as bass
import concourse.mybir as mybir
from concourse.bass_utils import run_bass_kernel_spmd

F32 = mybir.dt.float32
BF16 = mybir.dt.bfloat16
U32 = mybir.dt.uint32
AF = mybir.ActivationFunctionType
ALU = mybir.AluOpType
AX = mybir.AxisListType

D = 1024
NT = 256
TOK = 4096
NTL = TOK // NT
NPRE = TOK // NT
JG = 4
NJG = 128 // JG
TB = 8
EPS = 1e-6
GELU_TANH = True
GELU_MANUAL = False
NO_INTERLEAVE = False
PRE_PULL = 0
STRICT_SYNC = False

K_G1, K_CAW, K_CAB, K_BA, K_BX, K_LAM, K_CBW, K_G2, K_GF, NPARAM = 0, 1, 5, 6, 7, 8, 9, 12, 13, 14


class _Rec:
    def __init__(self):
        self.call = None

    def __getattr__(self, name):
        def f(*a, **kw):
            self.call = (name, a, kw)
            return self
        return f


class _Op:
    __slots__ = ("e", "call", "deps", "need", "dma", "idx", "sigval")


class Sch:
    COMPUTE = ('pe', 'act', 'dve', 'pool')

    def __init__(self, nc, es):
        self.nc = nc
        self.es = es
        self.eng = {'pe': nc.tensor, 'act': nc.scalar, 'dve': nc.vector, 'pool': nc.gpsimd, 'sp': nc.sync}
        self.sems = {}
        self.sigcnt = {}
        self.issue = {}
        for e in self.eng:
            self.sems[e] = es.enter_context(nc.semaphore("s_" + e))
            self.sigcnt[e] = 0
            self.issue[e] = 0
        self.dcnt = {}
        self.waited = {e: {} for e in self.eng}
        self.lastw = {}
        self.readers = {}
        self.ops = []
        self.lastop = {}

    def op(self, e, fn, r=(), w=(), dma=None, sig=True):
        rec = _Rec()
        fn(rec)
        o = _Op()
        o.e = e
        o.call = rec.call
        o.need = False
        o.dma = dma
        o.sigval = None
        self.issue[e] += 1
        o.idx = self.issue[e]
        deps = {}

        def add(x):
            if x is None:
                return
            stream, idx, prod = x
            if stream == e:
                if e == 'pe' or (idx < o.idx - 1 and not STRICT_SYNC):
                    return
            if stream in deps and deps[stream][0] >= idx:
                return
            deps[stream] = (idx, prod)

        for k in r:
            add(self.lastw.get(k))
        for k in w:
            add(self.lastw.get(k))
            for x in self.readers.get(k, {}).values():
                add(x)
        o.deps = []
        for stream, (idx, prod) in deps.items():
            if self.waited[e].get(stream, 0) >= idx:
                continue
            self.waited[e][stream] = idx
            o.deps.append((stream, idx, prod))
            if prod is not None:
                prod.need = True
        if dma is not None:
            if dma not in self.sems:
                self.sems[dma] = self.es.enter_context(self.nc.semaphore("d_" + dma))
                self.dcnt[dma] = 0
            self.dcnt[dma] += 1
            me = (dma, self.dcnt[dma], None)
        else:
            me = (e, o.idx, o)
            self.lastop[e] = o
        for k in r:
            d = self.readers.setdefault(k, {})
            if me[0] not in d or d[me[0]][1] < me[1]:
                d[me[0]] = me
        for k in w:
            self.lastw[k] = me
            self.readers[k] = {}
        self.ops.append(o)

    def flush(self):
        for o in self.ops:
            if o.dma is None and o.need:
                self.sigcnt[o.e] += 1
                o.sigval = self.sigcnt[o.e]
        for o in self.ops:
            E = self.eng[o.e]
            for stream, idx, prod in o.deps:
                if prod is None:
                    E.wait_ge(self.sems[stream], 16 * idx)
                else:
                    E.wait_ge(self.sems[stream], prod.sigval)
            name, a, kw = o.call
            ins = getattr(E, name)(*a, **kw)
            if o.dma is not None:
                ins.then_inc(self.sems[o.dma], 16)
            elif o.need:
                ins.then_inc(self.sems[o.e], 1)
        self.ops = []

    def barrier(self):
        for e in self.COMPUTE:
            o = self.lastop.get(e)
            if o is not None and o.sigval is None:
                o.need = True
        self.flush()
        for e, E in self.eng.items():
            for s2 in self.COMPUTE:
                if self.sigcnt[s2] > 0 and (s2 != e or (STRICT_SYNC and e != 'pe')):
                    E.wait_ge(self.sems[s2], self.sigcnt[s2])
                self.waited[e][s2] = self.issue[s2]
            for d, c in self.dcnt.items():
                if c > 0:
                    E.wait_ge(self.sems[d], 16 * c)
                self.waited[e][d] = c
        self.lastw = {}
        self.readers = {}
        self.lastop = {}


def build_nc(NPRE=NPRE, NTL=NTL):
    nc = bass.Bass("TRN2", target_bir_lowering=False)

    def din(name, shape):
        return nc.dram_tensor(name, list(shape), F32, kind="ExternalInput").ap()

    xl = din("xl", [NPRE + NTL, 128, 8 * NT])
    winl = din("winl", [7, 128, 8192])
    woutl = din("woutl", [128, 8192])
    wql = din("wql", [4, 128, 4096])
    subkl = din("subkl", [128, 2048])
    ul = din("ul", [NJG, 128, 8 * JG * 128])
    vl = din("vl", [NJG, 128, JG * 1024])
    wal = din("wal", [128, 1024])
    wxl = din("wxl", [128, 1024])
    chpl = din("chpl", [128, 8 * NPARAM])
    identl = din("identl", [128, 128])
    iotal = din("iotal", [128, 128])
    onesl = din("onesl", [128, 128])
    hflagl = din("hflagl", [128, 1])
    outl = nc.dram_tensor("outl", [NTL, 128, 8 * NT], F32, kind="ExternalOutput").ap()
    win_bf = nc.dram_tensor("win_bf", [7, 128, 8192], BF16, kind="Internal").ap()
    wq_bf = nc.dram_tensor("wq_bf", [4, 128, 4096], BF16, kind="Internal").ap()
    u_bf = nc.dram_tensor("u_bf", [NJG, 128, 8 * JG * 128], BF16, kind="Internal").ap()
    v_bf = nc.dram_tensor("v_bf", [NJG, 128, JG * 1024], BF16, kind="Internal").ap()

    with ExitStack() as es:
        S = Sch(nc, es)

        uid = [0]

        def sb(stack, name, shape, dt):
            uid[0] += 1
            return stack.enter_context(nc.sbuf_tensor(f"{name}_{uid[0]}", list(shape), dt))

        def cap(t, off, dims):
            full = t[:]
            ps = full.ap[0][0]
            return bass.AP(full.tensor, off, [[ps, 128]] + [list(d) for d in dims])

        PS = [es.enter_context(nc.psum_tensor(f"bank{i}", [128, 512], F32)) for i in range(8)]
        wout = sb(es, "wout", [128, 8192], BF16)
        subk = sb(es, "subk", [128, 2048], BF16)
        wa = sb(es, "wa", [128, 1024], BF16)
        wx = sb(es, "wx", [128, 1024], BF16)
        ident = sb(es, "ident", [128, 128], F32)
        iota = sb(es, "iota", [128, 128], F32)
        ones = sb(es, "ones", [128, 128], F32)
        chp = sb(es, "chp", [128, 8 * NPARAM], F32)
        hflag = sb(es, "hflag", [128, 1], F32)
        c8 = sb(es, "c8", [128, 8], F32)
        tmp8 = sb(es, "tmp8", [128, 8], F32)
        xa_halo = sb(es, "xa_halo", [128, 8 * 3], F32)
        cv_halo = sb(es, "cv_halo", [128, 8 * 2], F32)
        hst = sb(es, "hst", [128, 8], F32)
        qT = sb(es, "qT", [128, 16 * NT], BF16)
        hT = [sb(es, f"hT{i}", [128, 8 * NT], F32) for i in range(2)]
        xn2 = [sb(es, f"xn2_{i}", [128, 8 * NT], BF16) for i in range(2)]
        Wt = sb(es, "Wt", [128, NT * 128], BF16)
        iT = sb(es, "iT", [128, NT], F32)
        jT = sb(es, "jT", [128, NT], F32)
        gT = sb(es, "gT", [128, NT], F32)
        sq = [sb(es, f"sq{i}", [128, NT], F32) for i in range(2)]
        rt = sb(es, "rt", [128, NT], F32)
        rstd = sb(es, "rstd", [128, NT], F32)
        c8h = sb(es, "c8h", [128, 8], F32)
        iota_bf = sb(es, "iota_bf", [128, 128], BF16)
        hba = sb(es, "hba", [128, 8], F32)
        hbx = sb(es, "hbx", [128, 8], F32)

        woutv = wout[:].rearrange("p (k n) -> p k n", k=8)
        subkv = subk[:].rearrange("p (h n) -> p h n", h=16)
        wav = wa[:].rearrange("p (h n) -> p h n", h=8)
        wxv = wx[:].rearrange("p (h n) -> p h n", h=8)
        chpv = chp[:].rearrange("p (c k) -> p c k", c=8)
        xav = xa_halo[:].rearrange("p (c k) -> p c k", c=8)
        cvv = cv_halo[:].rearrange("p (c k) -> p c k", c=8)
        qTv = qT[:].rearrange("p (h t) -> p h t", h=16)
        hTvs = [t[:].rearrange("p (c t) -> p c t", c=8) for t in hT]
        xn2v = [t[:].rearrange("p (c t) -> p c t", c=8) for t in xn2]

        def par(c, k):
            return chpv[:, c, k:k + 1]

        bank_rr = [0]

        def nb():
            bank_rr[0] = bank_rr[0] % 7 + 1
            return bank_rr[0]

        def act_copy(out, in_, r, w):
            S.op('act', lambda E: E.activation(out=out, in_=in_, func=AF.Identity), r=r, w=w)

        def gelu_from(src, srckeys, dst, dstkey, tmps, tmpkeys, post=None):
            if not GELU_MANUAL:
                fn = AF.Gelu_apprx_tanh if GELU_TANH else AF.Gelu
                if post is None:
                    S.op('act', lambda E: E.activation(out=dst, in_=src, func=fn), r=srckeys, w=[dstkey])
                else:
                    pap, pkey = post
                    S.op('act', lambda E: E.activation(out=tmps[0], in_=src, func=fn), r=srckeys, w=[tmpkeys[0]])
                    S.op('dve', lambda E: E.tensor_tensor(out=dst, in0=tmps[0], in1=pap, op=ALU.mult),
                         r=[tmpkeys[0], pkey], w=[dstkey])
                return
            t0, t1 = tmps
            k0, k1 = tmpkeys
            S.op('act', lambda E: E.activation(out=t0, in_=src, func=AF.Square), r=srckeys, w=[k0])
            S.op('pool', lambda E: E.tensor_scalar(out=t0, in0=t0, scalar1=0.044715, scalar2=1.0, op0=ALU.mult,
                                                   op1=ALU.add), r=[k0], w=[k0])
            S.op('dve', lambda E: E.tensor_tensor(out=t0, in0=src, in1=t0, op=ALU.mult), r=srckeys + [k0], w=[k0])
            S.op('act', lambda E: E.activation(out=t0, in_=t0, func=AF.Sigmoid, scale=1.5957691216057308),
                 r=[k0], w=[k0])
            if post is None:
                S.op('dve', lambda E: E.tensor_tensor(out=dst, in0=src, in1=t0, op=ALU.mult), r=srckeys + [k0], w=[dstkey])
            else:
                pap, pkey = post
                S.op('dve', lambda E: E.tensor_tensor(out=t1, in0=src, in1=t0, op=ALU.mult), r=srckeys + [k0], w=[k1])
                S.op('pool', lambda E: E.tensor_tensor(out=dst, in0=t1, in1=pap, op=ALU.mult), r=[k1, pkey], w=[dstkey])

        for (dst, src, key) in [(chp, chpl, 'chp'), (ident, identl, 'ident'), (iota, iotal, 'iota'),
                                (ones, onesl, 'ones'), (hflag, hflagl, 'hflag')]:
            S.op('sp', lambda E: E.dma_start(out=dst[:], in_=src[:, :]), w=[key], dma='cst_' + key)
        S.op('sp', lambda E: E.dma_start(out=hT[0][:], in_=xl[0]), w=['hT0'], dma='dx0')
        S.op('pool', lambda E: E.memset(xa_halo[:], 0.0), w=['xa_halo'])
        S.op('pool', lambda E: E.memset(cv_halo[:], 0.0), w=['cv_halo'])
        S.op('pool', lambda E: E.memset(hst[:], 0.0), w=['hst'])
        S.op('act', lambda E: E.activation(out=tmp8[:], in_=chpv[:, :, K_LAM], func=AF.Exp, scale=-1.0),
             r=['chp'], w=['tmp8'])
        S.op('act', lambda E: E.activation(out=tmp8[:], in_=tmp8[:], func=AF.Ln, bias=1.0, scale=1.0),
             r=['tmp8'], w=['tmp8'])
        S.op('dve', lambda E: E.tensor_copy(out=iota_bf[:], in_=iota[:]), r=['iota'], w=['iota_bf'])
        S.op('dve', lambda E: E.tensor_scalar(out=c8h[:], in0=tmp8[:], scalar1=-4.0, scalar2=None, op0=ALU.mult),
             r=['tmp8'], w=['c8h'])
        S.op('dve', lambda E: E.tensor_scalar(out=hba[:], in0=chpv[:, :, K_BA], scalar1=0.5, scalar2=None, op0=ALU.mult),
             r=['chp'], w=['hba'])
        S.op('dve', lambda E: E.tensor_scalar(out=hbx[:], in0=chpv[:, :, K_BX], scalar1=0.5, scalar2=None, op0=ALU.mult),
             r=['chp'], w=['hbx'])

        with ExitStack() as pst, nc.named_scope("prepass"):
            NST = 3
            st32 = [sb(pst, f"st32_{i}", [128, 4096], F32) for i in range(NST)]
            st16 = [sb(pst, f"st16_{i}", [128, 4096], BF16) for i in range(NST)]
            pieces = []
            pieces.append((woutl[:, 0:4096], None, (wout[:, 0:4096], 'wout'), 4096))
            pieces.append((woutl[:, 4096:8192], None, (wout[:, 4096:8192], 'wout'), 4096))
            pieces.append((subkl[:, :], None, (subk[:], 'subk'), 2048))
            pieces.append((wal[:, :], None, (wa[:], 'wa'), 1024))
            pieces.append((wxl[:, :], None, (wx[:], 'wx'), 1024))
            for g in range(7):
                for hh_ in range(2):
                    pieces.append((winl[g][:, hh_ * 4096:(hh_ + 1) * 4096], win_bf[g][:, hh_ * 4096:(hh_ + 1) * 4096],
                                   None, 4096))
            for q in range(4):
                pieces.append((wql[q], wq_bf[q], None, 4096))
            for g in range(NJG):
                pieces.append((ul[g], u_bf[g], None, 4096))
                pieces.append((vl[g], v_bf[g], None, 4096))
            cengs = ['dve', 'act']

            def emit_load(i):
                src, ddst, sdst, n = pieces[i]
                b = i % NST
                S.op('sp', lambda E: E.dma_start(out=st32[b][:, :n], in_=src), w=[f'st32_{b}'], dma=f'pin{b}')

            for i in range(min(NST - 1, len(pieces))):
                emit_load(i)
            for i, (src, ddst, sdst, n) in enumerate(pieces):
                b = i % NST
                ce = cengs[i % 2]
                if sdst is not None:
                    o, okey = sdst
                else:
                    o, okey = st16[b][:, :n], f'st16_{b}'
                if ce == 'act':
                    S.op('act', lambda E: E.activation(out=o, in_=st32[b][:, :n], func=AF.Identity),
                         r=[f'st32_{b}'], w=[okey])
                else:
                    S.op(ce, lambda E: E.tensor_copy(out=o, in_=st32[b][:, :n]), r=[f'st32_{b}'], w=[okey])
                if i + NST - 1 < len(pieces):
                    emit_load(i + NST - 1)
                if ddst is not None:
                    S.op('sp', lambda E: E.dma_start(out=ddst, in_=st16[b][:, :n]), r=[okey], dma=f'pout{b}')
            S.barrier()

        def rmsnorm(srcv, skey, gk, dstv, dkey, slot=None, slotkey='b0'):
            if slot is None:
                slot = PS[0][:, :NT]
            for c in range(8):
                S.op('act', lambda E: E.activation(out=sq[c % 2][:], in_=srcv[:, c, :], func=AF.Square),
                     r=[skey], w=[f'sq{c % 2}'])
                S.op('pe', lambda E: E.matmul(slot, lhsT=ones[:], rhs=sq[c % 2][:],
                                              start=(c == 0), stop=(c == 7)),
                     r=['ones', f'sq{c % 2}'], w=[slotkey])
            S.op('act', lambda E: E.activation(out=rt[:], in_=slot, func=AF.Sqrt, bias=EPS, scale=1.0 / D),
                 r=[slotkey], w=['rt'])
            S.op('dve', lambda E: E.reciprocal(out=rstd[:], in_=rt[:]), r=['rt'], w=['rstd'])
            for c in range(8):
                S.op('dve', lambda E: E.scalar_tensor_tensor(out=dstv[:, c, :], in0=srcv[:, c, :], scalar=par(c, gk),
                                                             in1=rstd[:], op0=ALU.mult, op1=ALU.mult),
                     r=[skey, 'rstd', 'chp'], w=[dkey])

        def alloc_M(stk):
            M = {}
            M['win'] = [sb(stk, f"win{i}", [128, 8192], BF16) for i in range(2)]
            M['XA'] = sb(stk, "m_XA", [128, 8 * (NT + 4)], F32)
            M['CV'] = sb(stk, "m_CV", [128, 8 * (NT + 4)], F32)
            for nm in ['B2', 'B3', 'B4']:
                M[nm] = sb(stk, "m_" + nm, [128, 8 * NT], F32)
            M['xcb'] = sb(stk, "m_xcb", [128, 8 * NT], BF16)
            M['merged'] = M['xcb']
            M['wstate'] = {'g': [None, None], 'n': 0}
            return M

        def phase_M(ti, full, lastpre, M):
            hTv = hTvs[ti % 2]
            khT = f'hT{ti % 2}'
            xb = hTv
            kx = khT
            xnv = xn2v[ti % 2]
            kxn = f'xn2_{ti % 2}'

            def next_x():
                if ti + 1 < NPRE + NTL and ti <= NPRE:
                    nb_ = (ti + 1) % 2
                    S.op('sp', lambda E: E.dma_start(out=hT[nb_][:], in_=xl[ti + 1]), w=[f'hT{nb_}'], dma=f'dx{nb_}')

            next_x()
            needall = full or lastpre
            winv = [w_[:].rearrange("p (d n) -> p d n", d=8) for w_ in M['win']]
            ws = M['wstate']
            XA = M['XA'][:].rearrange("p (c t) -> p c t", c=8)
            CV = M['CV'][:].rearrange("p (c t) -> p c t", c=8)
            XC = M['B2'][:].rearrange("p (c t) -> p c t", c=8)
            RA = M['B3'][:].rearrange("p (c t) -> p c t", c=8)
            IG = M['B4'][:].rearrange("p (c t) -> p c t", c=8)
            T1 = XA[:, :, 0:NT]
            xcbv = M['xcb'][:].rearrange("p (c t) -> p c t", c=8)
            mergedv = M['merged'][:].rearrange("p (c t) -> p c t", c=8)
            glist = [0] + ([2, 4] if needall else []) + ([3, 1, 5, 6] if full else [])

            def ensure_w(g):
                for b in range(2):
                    if ws['g'][b] == g:
                        return b
                b = ws['n'] % 2
                ws['n'] += 1
                ws['g'][b] = g
                S.op('sp', lambda E: E.dma_start(out=M['win'][b][:], in_=win_bf[g]), w=[f'win{b}'], dma=f'dw{b}')
                return b

            def proj(g, c):
                b = ensure_w(g)
                bk = nb()
                for dc in range(8):
                    S.op('pe', lambda E: E.matmul(PS[bk][:, :NT], lhsT=winv[b][:, dc, c * 128:(c + 1) * 128],
                                                  rhs=xnv[:, dc, :], start=(dc == 0), stop=(dc == 7)),
                         r=[f'win{b}', kxn], w=[f'b{bk}'])
                return bk

            def stage_begin(g):
                ensure_w(g)
                i_ = glist.index(g)
                if i_ + 1 < len(glist):
                    ensure_w(glist[i_ + 1])

            ensure_w(glist[0])
            rmsnorm(xb, kx, K_G1, xnv, kxn)
            stage_begin(0)
            S.op('pool', lambda E: E.tensor_copy(out=XA[:, :, 0:3], in_=xav), r=['xa_halo'], w=['XA'])
            for c in range(8):
                bk = proj(0, c)
                act_copy(XA[:, c, 3:3 + NT], PS[bk][:, :NT], [f'b{bk}'], ['XA'])
            S.op('pool', lambda E: E.tensor_copy(out=xav, in_=XA[:, :, NT:NT + 3]), r=['XA'], w=['xa_halo'])
            for c in range(8):
                S.op('dve', lambda E: E.tensor_scalar(out=XC[:, c, :], in0=XA[:, c, 0:NT], scalar1=par(c, K_CAW),
                                                      scalar2=par(c, K_CAB), op0=ALU.mult, op1=ALU.add),
                     r=['XA', 'chp'], w=['B2'])
            for k in range(1, 4):
                for c in range(8):
                    S.op('dve', lambda E: E.scalar_tensor_tensor(out=XC[:, c, :], in0=XA[:, c, k:k + NT],
                                                                 scalar=par(c, K_CAW + k), in1=XC[:, c, :],
                                                                 op0=ALU.mult, op1=ALU.add),
                         r=['XA', 'chp', 'B2'], w=['B2'])
            act_copy(M['xcb'][:], M['B2'][:], ['B2'], ['xcb'])
            for c in range(8):
                b1 = nb()
                S.op('pe', lambda E: E.matmul(PS[b1][:, :NT], lhsT=wav[:, c, :], rhs=xcbv[:, c, :], start=True, stop=True),
                     r=['wa', 'xcb'], w=[f'b{b1}'])
                b2 = nb()
                S.op('pe', lambda E: E.matmul(PS[b2][:, :NT], lhsT=wxv[:, c, :], rhs=xcbv[:, c, :], start=True, stop=True),
                     r=['wx', 'xcb'], w=[f'b{b2}'])
                S.op('act', lambda E: E.activation(out=RA[:, c, :], in_=PS[b1][:, :NT], func=AF.Tanh,
                                                   bias=hba[:, c:c + 1], scale=0.5),
                     r=[f'b{b1}', 'hba'], w=['B3'])
                S.op('act', lambda E: E.activation(out=IG[:, c, :], in_=PS[b2][:, :NT], func=AF.Tanh,
                                                   bias=hbx[:, c:c + 1], scale=0.5),
                     r=[f'b{b2}', 'hbx'], w=['B4'])
            for c in range(8):
                S.op('act', lambda E: E.activation(out=RA[:, c, :], in_=RA[:, c, :], func=AF.Exp,
                                                   bias=c8h[:, c:c + 1], scale=c8h[:, c:c + 1]),
                     r=['B3', 'c8h'], w=['B3'])
            S.op('act', lambda E: E.activation(out=T1, in_=RA, func=AF.Square), r=['B3'], w=['XA'])
            S.op('dve', lambda E: E.tensor_scalar(out=T1, in0=T1, scalar1=1.0, scalar2=-1.0,
                                                  op0=ALU.min, op1=ALU.mult), r=['XA'], w=['XA'])
            S.op('dve', lambda E: E.tensor_scalar(out=M['B4'][:], in0=M['B4'][:], scalar1=0.5, scalar2=0.5,
                                                  op0=ALU.mult, op1=ALU.add), r=['B4'], w=['B4'])
            S.op('act', lambda E: E.activation(out=T1, in_=T1, func=AF.Sqrt, bias=1.0, scale=1.0),
                 r=['XA'], w=['XA'])
            S.op('dve', lambda E: E.tensor_tensor(out=M['B2'][:], in0=M['B2'][:], in1=M['B4'][:], op=ALU.mult),
                 r=['B2', 'B4'], w=['B2'])
            S.op('dve', lambda E: E.tensor_tensor(out=XC, in0=XC, in1=T1, op=ALU.mult),
                 r=['B2', 'XA'], w=['B2'])
            for c in range(8):
                S.op('dve', lambda E: E.tensor_tensor_scan(out=T1[:, c, :], data0=RA[:, c, :], data1=XC[:, c, :],
                                                           initial=hst[:, c:c + 1], op0=ALU.mult, op1=ALU.add),
                     r=['B3', 'B2', 'hst'], w=['XA'])
            if lastpre:
                S.op('pool', lambda E: E.tensor_scalar(out=hst[:], in0=T1[:, :, NT - 1], scalar1=hflag[:, 0:1],
                                                       scalar2=None, op0=ALU.mult),
                     r=['XA', 'hflag'], w=['hst'])
            else:
                S.op('pool', lambda E: E.tensor_copy(out=hst[:], in_=T1[:, :, NT - 1]), r=['XA'], w=['hst'])
            if not needall:
                return
            VB = XC
            stage_begin(2)
            for c in range(8):
                bk = proj(2, c)
                act_copy(VB[:, c, :], PS[bk][:, :NT], [f'b{bk}'], ['B2'])
            S.op('pool', lambda E: E.tensor_copy(out=CV[:, :, 0:2], in_=cvv), r=['cv_halo'], w=['CV'])
            stage_begin(4)
            for c in range(8):
                bk = proj(4, c)
                S.op('dve', lambda E: E.tensor_tensor(out=CV[:, c, 2:2 + NT], in0=PS[bk][:, :NT], in1=VB[:, c, :],
                                                      op=ALU.mult),
                     r=[f'b{bk}', 'B2'], w=['CV'])
            S.op('pool', lambda E: E.tensor_copy(out=cvv, in_=CV[:, :, NT:NT + 2]), r=['CV'], w=['cv_halo'])
            if not full:
                return
            YCV = RA
            for c in range(8):
                S.op('dve', lambda E: E.tensor_scalar(out=YCV[:, c, :], in0=CV[:, c, 0:NT], scalar1=par(c, K_CBW),
                                                      scalar2=None, op0=ALU.mult),
                     r=['CV', 'chp'], w=['B3'])
            for k in range(1, 3):
                for c in range(8):
                    S.op('dve', lambda E: E.scalar_tensor_tensor(out=YCV[:, c, :], in0=CV[:, c, k:k + NT],
                                                                 scalar=par(c, K_CBW + k), in1=YCV[:, c, :],
                                                                 op0=ALU.mult, op1=ALU.add),
                         r=['CV', 'chp', 'B3'], w=['B3'])
            stage_begin(3)
            for c in range(8):
                bk = proj(3, c)
                S.op('dve', lambda E: E.tensor_tensor(out=YCV[:, c, :], in0=PS[bk][:, :NT], in1=YCV[:, c, :], op=ALU.mult),
                     r=[f'b{bk}', 'B3'], w=['B3'])
            GG = IG
            stage_begin(1)
            for c in range(8):
                bk = proj(1, c)
                S.op('act', lambda E: E.activation(out=GG[:, c, :], in_=PS[bk][:, :NT], func=AF.Gelu_apprx_tanh),
                     r=[f'b{bk}'], w=['B4'])
            SA = CV
            SB = XC
            stage_begin(5)
            for c in range(8):
                bk = proj(5, c)
                S.op('act', lambda E: E.activation(out=SA[:, c, 0:NT], in_=PS[bk][:, :NT], func=AF.Tanh, scale=0.5),
                     r=[f'b{bk}'], w=['CV'])
            stage_begin(6)
            for c in range(8):
                bk = proj(6, c)
                S.op('act', lambda E: E.activation(out=SB[:, c, :], in_=PS[bk][:, :NT], func=AF.Tanh, scale=0.5),
                     r=[f'b{bk}'], w=['B2'])
            S.op('dve', lambda E: E.tensor_tensor(out=GG, in0=GG, in1=T1, op=ALU.mult), r=['B4', 'XA'], w=['B4'])
            S.op('dve', lambda E: E.tensor_scalar(out=SA[:, :, 0:NT], in0=SA[:, :, 0:NT], scalar1=0.5, scalar2=0.5,
                                                  op0=ALU.mult, op1=ALU.add), r=['CV'], w=['CV'])
            S.op('dve', lambda E: E.tensor_scalar(out=SB, in0=SB, scalar1=0.5, scalar2=0.5,
                                                  op0=ALU.mult, op1=ALU.add), r=['B2'], w=['B2'])
            S.op('dve', lambda E: E.tensor_tensor(out=GG, in0=GG, in1=SA[:, :, 0:NT], op=ALU.mult),
                 r=['B4', 'CV'], w=['B4'])
            S.op('dve', lambda E: E.tensor_tensor(out=YCV, in0=YCV, in1=SB, op=ALU.mult), r=['B3', 'B2'], w=['B3'])
            S.op('dve', lambda E: E.tensor_tensor(out=mergedv, in0=GG, in1=YCV, op=ALU.add),
                 r=['B4', 'B3'], w=['xcb'])
            for oc in range(8):
                bk = nb()
                for kc in range(8):
                    S.op('pe', lambda E: E.matmul(PS[bk][:, :NT], lhsT=woutv[:, kc, oc * 128:(oc + 1) * 128],
                                                  rhs=mergedv[:, kc, :], start=(kc == 0), stop=(kc == 7)),
                         r=['wout', 'xcb'], w=[f'b{bk}'])
                S.op('dve', lambda E: E.tensor_tensor(out=hTv[:, oc, :], in0=PS[bk][:, :NT], in1=hTv[:, oc, :], op=ALU.add),
                     r=[f'b{bk}', khT], w=[khT])
            for piece in range(4):
                b = piece // 2
                lo = (piece % 2) * 4096
                S.op('sp', lambda E: E.dma_start(out=M['win'][b][:, lo:lo + 4096], in_=wq_bf[piece]),
                     w=[f'win{b}'], dma=f'dw{b}')
            rmsnorm(hTv, khT, K_G2, xnv, kxn)
            for hp in range(16):
                piece, k = hp // 4, hp % 4
                b = piece // 2
                wq_ = M['win'][b][:, (piece % 2) * 4096 + k * 1024:(piece % 2) * 4096 + (k + 1) * 1024]
                wqv_ = wq_.rearrange("p (d n) -> p d n", d=8)
                bk = nb()
                for dc in range(8):
                    S.op('pe', lambda E: E.matmul(PS[bk][:, :NT], lhsT=wqv_[:, dc, :], rhs=xnv[:, dc, :],
                                                  start=(dc == 0), stop=(dc == 7)),
                         r=[f'win{b}', kxn], w=[f'b{bk}'])
                act_copy(qTv[:, hp, :], PS[bk][:, :NT], [f'b{bk}'], ['qT'])

        def alloc_P1a(stk):
            T = {}
            T['sc'] = sb(stk, "sc", [128, 2048], F32)
            T['m'] = sb(stk, "m", [128, 256], F32)
            T['iu'] = sb(stk, "iu", [128, 256], U32)
            T['idf'] = sb(stk, "idf", [128, 256], F32)
            T['wk1s'] = [sb(stk, f"wk1_{i}", [128, 128], F32) for i in range(2)]
            T['cand'] = sb(stk, "cand", [128, 2048], F32)
            T['wk2s'] = [sb(stk, f"wk2_{i}", [128, 256], F32) for i in range(2)]
            for nm, dt_ in [('c16', F32), ('pu', U32), ('au', U32), ('bu', U32), ('af', F32), ('bf_', F32),
                            ('e16', F32), ('gsel', F32), ('isel', F32), ('jsel', F32)]:
                T[nm] = sb(stk, nm, [128, 128], dt_)
            for nm in ['negmax', 'Z', 'rz']:
                T[nm] = sb(stk, nm, [128, 8], F32)
            return T

        def gen_P1a(ti, T, slots):
            par_ = ti % 2
            hTv = hTvs[par_]
            khT = f'hT{par_}'
            x2v = xn2v[par_]
            kx2 = f'xn2_{par_}'
            (nslot, nkey), xs = slots
            xi = [0]

            def nslot_():
                xi[0] = (xi[0] + 1) % len(xs)
                return xs[xi[0]]

            sc, m, iu, idf = T['sc'], T['m'], T['iu'], T['idf']
            wk1s, cand, wk2s = T['wk1s'], T['cand'], T['wk2s']
            c16, pu, au, bu, af, bf_ = T['c16'], T['pu'], T['au'], T['bu'], T['af'], T['bf_']
            negmax, Z, rz, e16, gsel, isel, jsel = T['negmax'], T['Z'], T['rz'], T['e16'], T['gsel'], T['isel'], T['jsel']
            scv = sc[:].rearrange("p (h n) -> p h n", h=16)
            mv = m[:].rearrange("p (h k) -> p h k", h=16)
            iuv = iu[:].rearrange("p (h k) -> p h k", h=16)
            candv = cand[:].rearrange("p (h a b) -> p h a b", h=8, a=16)
            cand3 = cand[:].rearrange("p (h ab) -> p h ab", h=8)
            c16v = c16[:].rearrange("p (h k) -> p h k", h=8)
            puv = pu[:].rearrange("p (h k) -> p h k", h=8)
            e16v = e16[:].rearrange("p (h k) -> p h k", h=8)
            gselv = gsel[:].rearrange("p (h k) -> p h k", h=8)
            PU = [f'pu{h}' for h in range(8)]
            C16 = [f'c16_{h}' for h in range(8)]

            for s in range(NT // 128):
                for hp4 in range(4):
                    for k2 in range(2):
                        sl, skeys = nslot_()
                        for k in range(2):
                            hp = hp4 * 4 + k2 * 2 + k
                            S.op('pe', lambda E: E.matmul(sl(k * 128, 128), lhsT=qTv[:, hp, s * 128:(s + 1) * 128],
                                                          rhs=subkv[:, hp, :], start=True, stop=True),
                                 r=['qT', 'subk'], w=skeys)
                        c0 = (hp4 * 4 + k2 * 2) * 128
                        act_copy(sc[:, c0:c0 + 256], sl(0, 256), skeys, ['sc'])
                    yield
                for hp0 in range(0, 16, 2):
                    pr = [(hp0, wk1s[0], 'wk1_0'), (hp0 + 1, wk1s[1], 'wk1_1')]
                    for hp, wk1, kk in pr:
                        S.op('dve', lambda E: E.max(out=mv[:, hp, 0:8], in_=scv[:, hp, :]), r=['sc'], w=[f'm{hp}'])
                    for hp, wk1, kk in pr:
                        S.op('dve', lambda E: E.max_index(out=iuv[:, hp, 0:8], in_max=mv[:, hp, 0:8],
                                                          in_values=scv[:, hp, :]), r=['sc', f'm{hp}'], w=[f'iu{hp}'])
                    for hp, wk1, kk in pr:
                        S.op('dve', lambda E: E.match_replace(out=wk1[:], in_to_replace=mv[:, hp, 0:8],
                                                              in_values=scv[:, hp, :], imm_value=-1e30),
                             r=['sc', f'm{hp}'], w=[kk])
                    for hp, wk1, kk in pr:
                        S.op('dve', lambda E: E.max(out=mv[:, hp, 8:16], in_=wk1[:]), r=[kk], w=[f'm{hp}'])
                    for hp, wk1, kk in pr:
                        S.op('dve', lambda E: E.max_index(out=iuv[:, hp, 8:16], in_max=mv[:, hp, 8:16], in_values=wk1[:]),
                             r=[kk, f'm{hp}'], w=[f'iu{hp}'])
                    yield
                S.op('dve', lambda E: E.tensor_copy(out=idf[:], in_=iu[:]), r=[f'iu{q}' for q in range(16)], w=['idf'])
                S.op('dve', lambda E: E.tensor_tensor(out=candv, in0=cap(m, 0, [[32, 8], [1, 16], [0, 16]]),
                                                      in1=cap(m, 16, [[32, 8], [0, 16], [1, 16]]), op=ALU.add),
                     r=[f'm{q}' for q in range(16)], w=['cand'])
                yield
                for h0 in range(0, 8, 2):
                    pr = [(h0, wk2s[0], 'wk2_0'), (h0 + 1, wk2s[1], 'wk2_1')]
                    for h, wk2, kk in pr:
                        S.op('dve', lambda E: E.max(out=c16v[:, h, 0:8], in_=cand3[:, h, :]), r=['cand'], w=[f'c16_{h}'])
                    for h, wk2, kk in pr:
                        S.op('dve', lambda E: E.max_index(out=puv[:, h, 0:8], in_max=c16v[:, h, 0:8],
                                                          in_values=cand3[:, h, :]), r=['cand', f'c16_{h}'], w=[f'pu{h}'])
                    for h, wk2, kk in pr:
                        S.op('dve', lambda E: E.match_replace(out=wk2[:], in_to_replace=c16v[:, h, 0:8],
                                                              in_values=cand3[:, h, :], imm_value=-1e30),
                             r=['cand', f'c16_{h}'], w=[kk])
                    for h, wk2, kk in pr:
                        S.op('dve', lambda E: E.max(out=c16v[:, h, 8:16], in_=wk2[:]), r=[kk], w=[f'c16_{h}'])
                    for h, wk2, kk in pr:
                        S.op('dve', lambda E: E.max_index(out=puv[:, h, 8:16], in_max=c16v[:, h, 8:16], in_values=wk2[:]),
                             r=[kk, f'c16_{h}'], w=[f'pu{h}'])
                    yield
                S.op('dve', lambda E: E.tensor_single_scalar(out=au[:], in_=pu[:], scalar=4, op=ALU.logical_shift_right),
                     r=PU, w=['au'])
                S.op('dve', lambda E: E.tensor_single_scalar(out=bu[:], in_=pu[:], scalar=15, op=ALU.bitwise_and),
                     r=PU, w=['bu'])
                S.op('dve', lambda E: E.tensor_copy(out=af[:], in_=au[:]), r=['au'], w=['af'])
                S.op('dve', lambda E: E.tensor_copy(out=bf_[:], in_=bu[:]), r=['bu'], w=['bf_'])
                S.op('dve', lambda E: E.tensor_scalar(out=negmax[:], in0=c16v[:, :, 0], scalar1=-1.0, scalar2=None,
                                                      op0=ALU.mult), r=C16, w=['negmax'])
                for h in range(8):
                    S.op('act', lambda E: E.activation(out=e16v[:, h, :], in_=c16v[:, h, :], func=AF.Exp,
                                                       bias=negmax[:, h:h + 1], scale=1.0),
                         r=[f'c16_{h}', 'negmax'], w=['e16'])
                yield
                S.op('dve', lambda E: E.tensor_reduce(out=Z[:], in_=e16v, axis=AX.X, op=ALU.add), r=['e16'], w=['Z'])
                S.op('dve', lambda E: E.reciprocal(out=rz[:], in_=Z[:]), r=['Z'], w=['rz'])
                S.op('dve', lambda E: E.tensor_tensor(out=gselv, in0=e16v, in1=cap(rz, 0, [[1, 8], [0, 16]]), op=ALU.mult),
                     r=['e16', 'rz'], w=['gsel'])
                oh3 = cand[:].rearrange("p (k a) -> p k a", a=16)
                oh4 = cand[:].rearrange("p (h k a) -> p h k a", h=8, k=16)
                for (sel, selkey, abf, abkey, off) in [(isel, 'isel', af, 'af', 0), (jsel, 'jsel', bf_, 'bf_', 16)]:
                    S.op('dve', lambda E: E.tensor_tensor(out=oh3, in0=cap(iota, 0, [[0, 128], [1, 16]]),
                                                          in1=cap(abf, 0, [[1, 128], [0, 16]]), op=ALU.is_equal),
                         r=['iota', abkey], w=['cand'])
                    yield
                    S.op('dve', lambda E: E.tensor_tensor(out=oh4, in0=oh4, in1=cap(idf, off, [[32, 8], [0, 16], [1, 16]]),
                                                          op=ALU.mult),
                         r=['cand', 'idf'], w=['cand'])
                    S.op('dve', lambda E: E.tensor_reduce(out=sel[:], in_=oh3, axis=AX.X, op=ALU.add),
                         r=['cand'], w=[selkey])
                    yield
                for (src, skey, dstT, dkey) in [(isel, 'isel', iT, 'iT'), (jsel, 'jsel', jT, 'jT'), (gsel, 'gsel', gT, 'gT')]:
                    sl, skeys = nslot_()
                    S.op('pe', lambda E: E.transpose(out=sl(0, 128), in_=src[:], identity=ident[:]),
                         r=[skey, 'ident'], w=skeys)
                    act_copy(dstT[:, s * 128:(s + 1) * 128], sl(0, 128), skeys, [dkey])
                yield

        def mkslot(bank, half=0):
            return (lambda off, n: PS[bank][:, off: off + n]), [f'b{bank}']

        def phase_WB(ti, stk):
            OH1 = [sb(stk, f"OH1_{i}", [128, TB * 128], BF16) for i in range(2)]
            OH2 = [sb(stk, f"OH2_{i}", [128, TB * 128], BF16) for i in range(2)]
            wbanks = [1, 2, 3, 4]
            for tb in range(NT // TB):
                t0 = tb * TB
                ob = tb % 2
                o1 = OH1[ob][:].rearrange("p (t i) -> p t i", t=TB)
                o2 = OH2[ob][:].rearrange("p (t i) -> p t i", t=TB)
                k1, k2 = f'OH1_{ob}', f'OH2_{ob}'
                for tt in range(TB):
                    t = t0 + tt
                    S.op('dve', lambda E: E.tensor_scalar(out=o1[:, tt, :], in0=iota_bf[:], scalar1=iT[:, t:t + 1],
                                                          scalar2=gT[:, t:t + 1], op0=ALU.is_equal, op1=ALU.mult),
                         r=['iota_bf', 'iT', 'gT'], w=[k1])
                    S.op('dve', lambda E: E.tensor_scalar(out=o2[:, tt, :], in0=iota_bf[:], scalar1=jT[:, t:t + 1],
                                                          scalar2=None, op0=ALU.is_equal),
                         r=['iota_bf', 'jT'], w=[k2])
                for tt in range(TB):
                    t = t0 + tt
                    bk = wbanks[(t // 4) % 4]
                    S.op('pe', lambda E: E.matmul(PS[bk][:, (t % 4) * 128:(t % 4 + 1) * 128], lhsT=o1[:, tt, :],
                                                  rhs=o2[:, tt, :], start=True, stop=True),
                         r=[k1, k2], w=[f'b{bk}'])
                    if t % 4 == 3:
                        act_copy(Wt[:, (t - 3) * 128:(t + 1) * 128], PS[bk][:, :512], [f'b{bk}'], ['Wt'])

        def phase_P2(ti, stk, gen):
            par_ = ti % 2
            hTv = hTvs[par_]
            khT = f'hT{par_}'
            x2v = xn2v[par_]
            kx2 = f'xn2_{par_}'
            U = [sb(stk, f"U{i}", [128, 8 * JG * 128], BF16) for i in range(2)]
            Vv = [sb(stk, f"V{i}", [128, JG * 1024], BF16) for i in range(2)]
            gt0 = [sb(stk, f"gt0_{i}", [128, NT], F32) for i in range(2)]
            Cb = [sb(stk, f"C{i}", [128, JG * NT], BF16) for i in range(2)]
            Uv = [u_[:].rearrange("p (d j i) -> p d j i", d=8, j=JG) for u_ in U]
            Vvv = [v_[:].rearrange("p (j d) -> p j d", j=JG) for v_ in Vv]
            Cv = [c_[:].rearrange("p (j t) -> p j t", j=JG) for c_ in Cb]
            aslots = [mkslot(0), mkslot(1)]

            def load(g):
                b = g % 2
                S.op('sp', lambda E: E.dma_start(out=U[b][:], in_=u_bf[g]), w=[f'U{b}'], dma=f'du{b}')
                S.op('sp', lambda E: E.dma_start(out=Vv[b][:], in_=v_bf[g]), w=[f'V{b}'], dma=f'dv{b}')

            def pull(n):
                if gen is None:
                    return
                for _ in range(n):
                    try:
                        next(gen)
                    except StopIteration:
                        return

            if NO_INTERLEAVE:
                pull(10000)
            else:
                pull(PRE_PULL)
            def loadU(g):
                b = g % 2
                S.op('sp', lambda E: E.dma_start(out=U[b][:], in_=u_bf[g]), w=[f'U{b}'], dma=f'du{b}')

            def loadV(g):
                b = g % 2
                S.op('sp', lambda E: E.dma_start(out=Vv[b][:], in_=v_bf[g]), w=[f'V{b}'], dma=f'dv{b}')

            def vside(g, dcs):
                b = g % 2
                for dc in dcs:
                    bk = 4 + dc // 2
                    lo = (dc % 2) * NT
                    for jj in range(JG):
                        S.op('pe', lambda E: E.matmul(PS[bk][:, lo:lo + NT], lhsT=Vvv[b][:, jj, dc * 128:(dc + 1) * 128],
                                                      rhs=Cv[b][:, jj, :],
                                                      start=(g == 0 and jj == 0 and dc % 2 == 0),
                                                      stop=(g == NJG - 1 and jj == JG - 1),
                                                      skip_group_check=True),
                             r=[f'V{b}', f'C{b}'], w=[f'pb{4 + dc // 2}'])

            loadU(0)
            loadV(0)
            loadV(1)
            na = 0
            for jg in range(NJG):
                if jg + 1 < NJG:
                    loadU(jg + 1)
                b = jg % 2
                for jj in range(JG):
                    j = jg * JG + jj
                    sl, skeys = aslots[na % 2]
                    na += 1
                    for dc in range(8):
                        S.op('pe', lambda E: E.matmul(sl(0, NT), lhsT=Uv[b][:, dc, jj, :], rhs=x2v[:, dc, :],
                                                      start=(dc == 0), stop=(dc == 7)),
                             r=[f'U{b}', kx2], w=skeys)
                    gb = jj % 2
                    S.op('act', lambda E: E.activation(out=gt0[gb][:], in_=sl(0, NT), func=AF.Gelu_apprx_tanh),
                         r=skeys, w=[f'gt0_{gb}'])
                    S.op('dve', lambda E: E.tensor_tensor(out=Cv[b][:, jj, :], in0=gt0[gb][:],
                                                          in1=cap(Wt, j, [[128, NT]]), op=ALU.mult),
                         r=[f'gt0_{gb}', 'Wt'], w=[f'C{b}'])
                    if jg > 0:
                        vside(jg - 1, [2 * jj, 2 * jj + 1])
                if jg > 0 and jg + 1 < NJG:
                    loadV(jg + 1)
                pull(2)
            vside(NJG - 1, list(range(8)))
            pull(10000)
            for dc in range(8):
                bk = 4 + dc // 2
                lo = (dc % 2) * NT
                S.op('dve', lambda E: E.tensor_tensor(out=hTv[:, dc, :], in0=PS[bk][:, lo:lo + NT], in1=hTv[:, dc, :],
                                                      op=ALU.add),
                     r=[f'pb{4 + dc // 2}', khT], w=[khT])
            rmsnorm(hTv, khT, K_GF, hTv, khT, slot=PS[2][:, :NT], slotkey='b2')
            S.op('sp', lambda E: E.dma_start(out=outl[ti - NPRE], in_=hT[par_][:]), r=[khT], dma='dout')
            if ti + 2 < NPRE + NTL:
                S.op('sp', lambda E: E.dma_start(out=hT[par_][:], in_=xl[ti + 2]), w=[khT], dma=f'dx{par_}')

        p1slots = ((PS[2][:, :NT], 'b2'), [mkslot(3)])
        with ExitStack() as stk:
            M = alloc_M(stk)
            for ti in range(NPRE):
                with nc.named_scope(f"pre{ti}"):
                    phase_M(ti, False, ti == NPRE - 1, M)
            S.barrier()
        t_first, t_last = NPRE, NPRE + NTL - 1
        with ExitStack() as stk:
            M = alloc_M(stk)
            with nc.named_scope(f"M{t_first}"):
                phase_M(t_first, True, False, M)
            S.barrier()
        with ExitStack() as stk:
            T = alloc_P1a(stk)
            with nc.named_scope(f"P1a_{t_first}"):
                for _ in gen_P1a(t_first, T, p1slots):
                    pass
            S.barrier()
        with ExitStack() as stk:
            with nc.named_scope(f"WB_{t_first}"):
                phase_WB(t_first, stk)
            S.barrier()
        for ti in range(t_first, t_last + 1):
            more = ti + 1 <= t_last
            if more:
                with ExitStack() as stk:
                    M = alloc_M(stk)
                    with nc.named_scope(f"M{ti + 1}"):
                        phase_M(ti + 1, True, False, M)
                    S.barrier()
            with ExitStack() as stk:
                gen = None
                if more:
                    T = alloc_P1a(stk)
                    gen = gen_P1a(ti + 1, T, p1slots)
                with nc.named_scope(f"OV_{ti}"):
                    phase_P2(ti, stk, gen)
                S.barrier()
            if more:
                with ExitStack() as stk:
                    with nc.named_scope(f"WB_{ti + 1}"):
                        phase_WB(ti + 1, stk)
                    S.barrier()
        nc.sync.wait_ge(S.sems['dout'], 16 * S.dcnt['dout'])
    return nc


_NC_CACHE = {}


def _layouts(x, norm1_g, w_in, conv_a_w, conv_a_b, w_a, b_a, w_x, b_x, lru_lambda, conv_b_w, w_out,
             norm2_g, peer_wq, peer_subkeys, peer_u, peer_v, final_g):
    f = np.float32
    shared = {}
    w = np.asarray(w_in[0], f).reshape(8, 128, 7, 1024)
    shared["winl"] = np.ascontiguousarray(w.transpose(2, 1, 0, 3)).reshape(7, 128, 8192)
    shared["woutl"] = np.ascontiguousarray(np.asarray(w_out[0], f).reshape(8, 128, 1024).transpose(1, 0, 2)).reshape(128, 8192)
    wq = np.asarray(peer_wq[0], f).reshape(8, 128, 4, 4, 128)
    shared["wql"] = np.ascontiguousarray(wq.transpose(2, 1, 3, 0, 4)).reshape(4, 128, 4096)
    sk = np.asarray(peer_subkeys[0], f).reshape(16, 128, 128)
    shared["subkl"] = np.ascontiguousarray(sk.transpose(2, 0, 1)).reshape(128, 2048)
    u = np.asarray(peer_u[0], f).reshape(128, NJG, JG, 8, 128)
    shared["ul"] = np.ascontiguousarray(u.transpose(1, 4, 3, 2, 0)).reshape(NJG, 128, 8 * JG * 128)
    v = np.asarray(peer_v[0], f).reshape(128, NJG, JG, 1024)
    shared["vl"] = np.ascontiguousarray(v.transpose(1, 0, 2, 3)).reshape(NJG, 128, JG * 1024)
    shared["wal"] = np.ascontiguousarray(np.asarray(w_a[0], f).transpose(1, 0, 2)).reshape(128, 1024)
    shared["wxl"] = np.ascontiguousarray(np.asarray(w_x[0], f).transpose(1, 0, 2)).reshape(128, 1024)
    chp = np.zeros((128, 8, NPARAM), f)

    def pc(vec):
        return np.asarray(vec, f).reshape(8, 128).T

    chp[:, :, K_G1] = pc(norm1_g[0])
    for k in range(4):
        chp[:, :, K_CAW + k] = pc(conv_a_w[0][k])
    chp[:, :, K_CAB] = pc(conv_a_b[0])
    chp[:, :, K_BA] = pc(b_a[0])
    chp[:, :, K_BX] = pc(b_x[0])
    chp[:, :, K_LAM] = pc(lru_lambda[0])
    for k in range(3):
        chp[:, :, K_CBW + k] = pc(conv_b_w[0][k])
    chp[:, :, K_G2] = pc(norm2_g[0])
    chp[:, :, K_GF] = pc(final_g)
    shared["chpl"] = chp.reshape(128, 8 * NPARAM)
    shared["identl"] = np.eye(128, dtype=f)
    shared["iotal"] = np.ascontiguousarray(np.broadcast_to(np.arange(128, dtype=f)[None, :], (128, 128)))
    shared["onesl"] = np.ones((128, 128), f)
    return shared


def kernel(x, norm1_g, w_in, conv_a_w, conv_a_b, w_a, b_a, w_x, b_x, lru_lambda, conv_b_w, w_out,
           norm2_g, peer_wq, peer_subkeys, peer_u, peer_v, final_g):
    f = np.float32
    x = np.asarray(x, f)
    B, Sq, _ = x.shape
    shared = _layouts(x, norm1_g, w_in, conv_a_w, conv_a_b, w_a, b_a, w_x, b_x, lru_lambda, conv_b_w, w_out,
                      norm2_g, peer_wq, peer_subkeys, peer_u, peer_v, final_g)
    in_maps = []
    for core in range(8):
        b, half = core // 2, core % 2
        win = np.zeros((2 * TOK, D), f)
        win[TOK:] = x[b, half * TOK:(half + 1) * TOK]
        if half == 1:
            win[:TOK] = x[b, 0:TOK]
        xl = np.ascontiguousarray(win.reshape(NPRE + NTL, NT, 8, 128).transpose(0, 3, 2, 1)).reshape(NPRE + NTL, 128, 8 * NT)
        m = dict(shared)
        m["xl"] = xl
        m["hflagl"] = np.full((128, 1), float(half), f)
        in_maps.append(m)
    if "nc" not in _NC_CACHE:
        _NC_CACHE["nc"] = build_nc()
    nc = _NC_CACHE["nc"]
    res = run_bass_kernel_spmd(nc, in_maps, core_ids=list(range(8)))
    out = np.zeros((B, Sq, D), f)
    for core in range(8):
        b, half = core // 2, core % 2
        o = np.asarray(res.results[core]["outl"], f).reshape(NTL, 128, 8, NT)
        o = o.transpose(0, 3, 2, 1).reshape(TOK, D)
        out[b, half * TOK:(half + 1) * TOK] = o
    return out
```
